# Optimizing a Trainium2 kernel written in Bass

```python
import math
import jax, jax.numpy as jnp
from jax import lax
import numpy as np

D_MODEL = 2048
BATCH = 4
SEQ = 2048
DEPTH = 2

ATT_HEAD_DIM = D_MODEL // 16
ATT_HEADS = 8
ATT_KV_HEADS = 2
ATT_WIDTH = ATT_HEADS * ATT_HEAD_DIM
ATT_KV_WIDTH = ATT_KV_HEADS * ATT_HEAD_DIM
WINDOW = 128
ATT_BLOCK = 128
ROPE_THETA = 10000.0
M_HEADS = 4
M_V_DIM = D_MODEL // 8
M_QK_DIM = M_V_DIM // 2
M_WIDTH = M_HEADS * M_V_DIM
M_QK_WIDTH = M_HEADS * M_QK_DIM
M_CHUNK = 64
MIX_WIDTH = ATT_WIDTH + M_WIDTH
IN_SIZES = (ATT_WIDTH, ATT_KV_WIDTH, ATT_KV_WIDTH, M_QK_WIDTH, M_QK_WIDTH, M_WIDTH, M_WIDTH, 4 * M_HEADS)
IN_WIDTH = ATT_WIDTH + 2 * ATT_KV_WIDTH + 2 * M_QK_WIDTH + 2 * M_WIDTH + 4 * M_HEADS
N_EXPERTS = 16
EXPERT_FF = D_MODEL // 2
CAPACITY_FACTOR = 2
EPS = 1e-6

kernel_name = "hymba_style_swa_mlstm_ec_moe_encoder"


def rms_norm(x):
    xf = x.astype(jnp.float32)
    return (xf * lax.rsqrt(jnp.mean(xf * xf, axis=-1, keepdims=True) + EPS)).astype(x.dtype)


def split_columns(t, sizes):
    offs = np.cumsum(np.array(sizes))[:-1].tolist()
    return jnp.split(t, offs, axis=-1)


def rope_tables(seq, dim, dtype):
    inv = 1.0 / (ROPE_THETA ** (jnp.arange(0, dim, 2, dtype=jnp.float32) / dim))
    ang = jnp.arange(seq, dtype=jnp.float32)[:, None] * inv[None, :]
    ang = jnp.concatenate([ang, ang], axis=-1)
    return jnp.cos(ang).astype(dtype), jnp.sin(ang).astype(dtype)


def apply_rope(x, cos, sin):
    x1, x2 = jnp.split(x, 2, axis=-1)
    rot = jnp.concatenate([-x2, x1], axis=-1)
    return x * cos[None, :, None, :] + rot * sin[None, :, None, :]


def windowed_gqa(q, k, v, sink):
    B, S, H, Dh = q.shape
    KV = k.shape[2]
    G = H // KV
    L = ATT_BLOCK
    nb = S // L
    qb = q.reshape(B, nb, L, KV, G, Dh)

    def band(t):
        tp = jnp.pad(t, ((0, 0), (L, L), (0, 0), (0, 0)))
        tb = tp.reshape(B, nb + 2, L, KV, Dh)
        return jnp.concatenate([tb[:, :-2], tb[:, 1:-1], tb[:, 2:]], axis=2)

    kb, vb = band(k), band(v)
    scores = jnp.einsum('bnqkgd,bnjkd->bnkgqj', qb, kb).astype(jnp.float32) * (Dh ** -0.5)
    blk = jnp.arange(nb)[:, None, None]
    qpos = blk * L + jnp.arange(L)[None, :, None]
    kpos = (blk - 1) * L + jnp.arange(3 * L)[None, None, :]
    mask = (jnp.abs(kpos - qpos) <= WINDOW) & (kpos >= 0) & (kpos < S)
    scores = jnp.where(mask[None, :, None, None], scores, jnp.finfo(jnp.float32).min)
    sink_col = jnp.broadcast_to(sink.astype(jnp.float32).reshape(1, 1, KV, G, 1, 1), scores.shape[:-1] + (1,))
    probs = jax.nn.softmax(jnp.concatenate([scores, sink_col], axis=-1), axis=-1)[..., :-1]
    out = jnp.einsum('bnkgqj,bnjkd->bnqkgd', probs.astype(v.dtype), vb)
    return out.reshape(B, S, H * Dh)


def mlstm_chunkwise(q, k, v, i_pre, f_pre):
    B, H, S, Dk = q.shape
    Dv = v.shape[-1]
    L = M_CHUNK
    nc = S // L
    q = q.astype(jnp.float32).reshape(B, H, nc, L, Dk) * (Dk ** -0.5)
    k = k.astype(jnp.float32).reshape(B, H, nc, L, Dk)
    v = v.astype(jnp.float32).reshape(B, H, nc, L, Dv)
    logf = jax.nn.log_sigmoid(f_pre.astype(jnp.float32)).reshape(B, H, nc, L)
    ig = i_pre.astype(jnp.float32).reshape(B, H, nc, L)
    b = jnp.cumsum(logf, axis=-1)
    b_tot = b[..., -1]
    lower = jnp.tril(jnp.ones((L, L), dtype=bool))
    dmat = jnp.where(lower, b[..., :, None] - b[..., None, :] + ig[..., None, :], -jnp.inf)
    w_state = b_tot[..., None] - b + ig
    m_loc = jnp.max(w_state, axis=-1)
    ek = jnp.exp(w_state - m_loc[..., None])[..., None] * k
    c_loc = jnp.einsum('bhclk,bhclv->bhckv', ek, v)
    n_loc = jnp.sum(ek, axis=3)

    def step(carry, inp):
        c_st, n_st, m_st = carry
        cl, nl, ml, bt = inp
        m_new = jnp.maximum(bt + m_st, ml)
        a = jnp.exp(bt + m_st - m_new)
        g = jnp.exp(ml - m_new)
        c_new = a[..., None, None] * c_st + g[..., None, None] * cl
        n_new = a[..., None] * n_st + g[..., None] * nl
        return (c_new, n_new, m_new), (c_st, n_st, m_st)

    init = (jnp.zeros((B, H, Dk, Dv), jnp.float32), jnp.zeros((B, H, Dk), jnp.float32),
            jnp.zeros((B, H), jnp.float32))
    xs = (jnp.moveaxis(c_loc, 2, 0), jnp.moveaxis(n_loc, 2, 0), jnp.moveaxis(m_loc, 2, 0), jnp.moveaxis(b_tot, 2, 0))
    _, (c_prev, n_prev, m_prev) = lax.scan(step, init, xs)
    c_prev = jnp.moveaxis(c_prev, 0, 2)
    n_prev = jnp.moveaxis(n_prev, 0, 2)
    m_prev = jnp.moveaxis(m_prev, 0, 2)
    g_inter = b + m_prev[..., None]
    m_t = jnp.maximum(jnp.max(dmat, axis=-1), g_inter)
    e_inter = jnp.exp(g_inter - m_t)
    s = jnp.einsum('bhctk,bhcsk->bhcts', q, k) * jnp.exp(dmat - m_t[..., None])
    num = jnp.einsum('bhcts,bhcsv->bhctv', s, v) + e_inter[..., None] * jnp.einsum('bhctk,bhckv->bhctv', q, c_prev)
    den = jnp.sum(s, axis=-1) + e_inter * jnp.einsum('bhctk,bhck->bhct', q, n_prev)
    h = num / jnp.maximum(jnp.abs(den), jnp.exp(-m_t))[..., None]
    return h.reshape(B, H, S, Dv)


def mlstm_bidirectional(q, k, v, i_fwd, f_fwd, i_bwd, f_bwd):
    flip = lambda t: jnp.flip(t, axis=2)
    h_fwd = mlstm_chunkwise(q, k, v, i_fwd, f_fwd)
    h_bwd = flip(mlstm_chunkwise(flip(q), flip(k), flip(v), flip(i_bwd), flip(f_bwd)))
    return h_fwd + h_bwd


def expert_choice_moe(h, w_router, w_gate, w_up, w_down):
    B, S, D = h.shape
    cap = CAPACITY_FACTOR * S // N_EXPERTS
    aff = jax.nn.softmax((h @ w_router).astype(jnp.float32), axis=-1)
    gates, idx = lax.top_k(jnp.swapaxes(aff, 1, 2), cap)
    xe = jax.vmap(lambda hb, ib: hb[ib])(h, idx)
    hid = jax.nn.silu(jnp.einsum('becd,edf->becf', xe, w_gate)) * jnp.einsum('becd,edf->becf', xe, w_up)
    ye = jnp.einsum('becf,efd->becd', hid, w_down) * gates[..., None].astype(h.dtype)
    flat_idx = (jnp.arange(B)[:, None, None] * S + idx).reshape(-1)
    out = jax.ops.segment_sum(ye.reshape(-1, D), flat_idx, num_segments=B * S)
    return out.reshape(B, S, D)


def hybrid_layer(x, c_act, w_ada, b_ada, w_in, b_gates, q_gain, k_gain, sink, m_gain, w_out,
                 w_router, w_gate, w_up, w_down, cos, sin):
    B, S, _ = x.shape
    mod = (c_act @ w_ada + b_ada)[:, None, :]
    sh1, sc1, g1, sh2, sc2, g2 = jnp.split(mod, 6, axis=-1)
    h = rms_norm(x) * (1 + sc1) + sh1
    aq, ak, av, mq, mk, mv, mo, mg = split_columns(h @ w_in, IN_SIZES)
    aq = rms_norm(aq.reshape(B, S, ATT_HEADS, ATT_HEAD_DIM)) * q_gain
    ak = rms_norm(ak.reshape(B, S, ATT_KV_HEADS, ATT_HEAD_DIM)) * k_gain
    av = av.reshape(B, S, ATT_KV_HEADS, ATT_HEAD_DIM)
    att_out = windowed_gqa(apply_rope(aq, cos, sin), apply_rope(ak, cos, sin), av, sink)
    to_heads = lambda t, d: jnp.transpose(t.reshape(B, S, M_HEADS, d), (0, 2, 1, 3))
    gate_pre = mg.astype(jnp.float32).reshape(B, S, 4, M_HEADS) + b_gates.astype(jnp.float32)
    gate_pre = jnp.transpose(gate_pre, (2, 0, 3, 1))
    hm = mlstm_bidirectional(to_heads(mq, M_QK_DIM), to_heads(mk, M_QK_DIM), to_heads(mv, M_V_DIM),
                             gate_pre[0], gate_pre[2], gate_pre[1], gate_pre[3])
    hm = rms_norm(jnp.transpose(hm, (0, 2, 1, 3))).astype(x.dtype) * m_gain.reshape(M_HEADS, M_V_DIM)
    m_out = hm.reshape(B, S, M_WIDTH) * jax.nn.sigmoid(mo)
    mix = jnp.concatenate([att_out, m_out], axis=-1) @ w_out
    x = x + g1 * mix
    h2 = rms_norm(x) * (1 + sc2) + sh2
    x = x + g2 * expert_choice_moe(h2, w_router, w_gate, w_up, w_down)
    return x


def setup_inputs(seed: int = 0) -> dict:
    key = jax.random.key(seed)
    ks = jax.random.split(key, 16)
    f32 = jnp.float32
    nrm = lambda k, shape, scale: jax.random.normal(k, shape, f32) * scale
    f_bias = jnp.linspace(3.0, 6.0, M_HEADS, dtype=f32)
    gate_noise = nrm(ks[5], (DEPTH, 4, M_HEADS), 0.1)
    b_gates = gate_noise + jnp.stack([jnp.zeros((M_HEADS,), f32), jnp.zeros((M_HEADS,), f32), f_bias, f_bias])[None]
    return {
        "x": nrm(ks[0], (BATCH, SEQ, D_MODEL), 1.0),
        "c": nrm(ks[1], (BATCH, D_MODEL), 1.0),
        "w_ada": nrm(ks[2], (DEPTH, D_MODEL, 6 * D_MODEL), 0.5 * D_MODEL ** -0.5),
        "b_ada": nrm(ks[3], (DEPTH, 6 * D_MODEL), 0.01),
        "w_in": nrm(ks[4], (DEPTH, D_MODEL, IN_WIDTH), D_MODEL ** -0.5),
        "b_gates": b_gates,
        "q_gain": 1.0 + nrm(ks[6], (DEPTH, ATT_HEAD_DIM), 0.1),
        "k_gain": 1.0 + nrm(ks[7], (DEPTH, ATT_HEAD_DIM), 0.1),
        "sink": nrm(ks[8], (DEPTH, ATT_HEADS), 1.0),
        "m_gain": 1.0 + nrm(ks[9], (DEPTH, M_WIDTH), 0.1),
        "w_out": nrm(ks[10], (DEPTH, MIX_WIDTH, D_MODEL), MIX_WIDTH ** -0.5),
        "w_router": nrm(ks[11], (DEPTH, D_MODEL, N_EXPERTS), D_MODEL ** -0.5),
        "w_gate": nrm(ks[12], (DEPTH, N_EXPERTS, D_MODEL, EXPERT_FF), D_MODEL ** -0.5),
        "w_up": nrm(ks[13], (DEPTH, N_EXPERTS, D_MODEL, EXPERT_FF), D_MODEL ** -0.5),
        "w_down": nrm(ks[14], (DEPTH, N_EXPERTS, EXPERT_FF, D_MODEL), EXPERT_FF ** -0.5),
    }


def reference(x, c, w_ada, b_ada, w_in, b_gates, q_gain, k_gain, sink, m_gain, w_out,
              w_router, w_gate, w_up, w_down):
    cos, sin = rope_tables(x.shape[1], ATT_HEAD_DIM, x.dtype)
    c_act = jax.nn.silu(c)
    for l in range(DEPTH):
        x = hybrid_layer(x, c_act, w_ada[l], b_ada[l], w_in[l], b_gates[l], q_gain[l], k_gain[l], sink[l],
                         m_gain[l], w_out[l], w_router[l], w_gate[l], w_up[l], w_down[l], cos, sin)
    return x
```

```python
import math
from contextlib import ExitStack
import numpy as np
import ml_dtypes
import concourse.bass as bass
import concourse.mybir as mybir
from concourse.bass_utils import run_bass_kernel_spmd

F32 = mybir.dt.float32
BF16 = mybir.dt.bfloat16
AF = mybir.ActivationFunctionType
ALU = mybir.AluOpType
AX = mybir.AxisListType
NPBF = ml_dtypes.bfloat16

D = 2048
S = 2048
NB = 4
DEPTH = 2
INW = 4624
NE = 16
FF = 1024
CAP = 256
EPS = 1e-6
ENGINES = ("tensor", "vector", "scalar", "gpsimd", "sync")


class Buf:
    __slots__ = ("name", "last_w", "readers", "dma_sem", "dma_cnt", "nowaw", "persistent")

    def __init__(self, name):
        self.name = name
        self.nowaw = False
        self.persistent = False
        self.last_w = None
        self.readers = []
        self.dma_sem = None
        self.dma_cnt = 0


class Op:
    __slots__ = ("eng", "fn", "is_dma", "deps", "signal", "semval", "dma_buf", "dma_val", "idx", "dma_inc", "phase", "dsem")


class Sched:
    def __init__(self, nc):
        self.nc = nc
        self.ops = []
        self.bufs = []
        self.phase = 0
        self.fence_start = 0

    def fence(self):
        deps = set()
        last = {}
        for o in self.ops[self.fence_start:]:
            if o.fn is None:
                continue
            if o.is_dma:
                deps.add(o.idx)
            else:
                last[o.eng] = o.idx
        deps.update(last.values())
        for e in ENGINES:
            o = Op()
            o.eng, o.fn, o.is_dma = e, None, False
            o.dma_inc = 16
            o.idx = len(self.ops)
            o.signal = False
            o.semval = None
            o.dma_buf = None
            o.dma_val = None
            o.dsem = None
            o.phase = self.phase
            o.deps = set(deps)
            self.ops.append(o)
        self.phase += 1
        self.fence_start = len(self.ops)

    def buf(self, name):
        b = Buf(name)
        self.bufs.append(b)
        return b

    def op(self, eng, fn, reads=(), writes=(), dma=False, dma_inc=16):
        o = Op()
        o.eng, o.fn, o.is_dma = eng, fn, dma
        o.dma_inc = dma_inc
        o.phase = self.phase
        o.dsem = None
        o.idx = len(self.ops)
        o.signal = False
        o.semval = None
        o.dma_buf = None
        o.dma_val = None
        deps = set()
        for b in reads:
            if b.last_w is not None:
                deps.add(b.last_w)
        for b in writes:
            if b.nowaw:
                continue
            if b.last_w is not None:
                deps.add(b.last_w)
            deps.update(b.readers)
        o.deps = deps
        for b in reads:
            b.readers.append(o.idx)
        for b in writes:
            b.last_w = o.idx
            b.readers = []
        if dma:
            assert len(writes) == 1
            o.dma_buf = writes[0]
        self.ops.append(o)
        return o

    def emit(self, final_wait_bufs=()):
        nc = self.nc
        ops = self.ops
        for o in ops:
            for d in o.deps:
                p = ops[d]
                if p.is_dma:
                    continue
                if p.eng == o.eng and p.eng == "tensor":
                    continue
                p.signal = True
        per_eng = {e: [] for e in ENGINES}
        for o in ops:
            per_eng[o.eng].append(o)
        cnt = {e: 0 for e in ENGINES}
        pers = {}
        pool_cnt = []
        phase_slots = {}
        cur_phase = -1
        sem_of = {}
        pers_cnt = []
        for o in ops:
            if o.fn is None:
                continue
            if o.is_dma:
                b = o.dma_buf
                if b.persistent:
                    if id(b) not in pers:
                        pers[id(b)] = len(pers_cnt)
                        pers_cnt.append(0)
                    sl = pers[id(b)]
                    pers_cnt[sl] += o.dma_inc
                    o.dma_val = pers_cnt[sl]
                    o.dsem = ("P", sl)
                else:
                    if o.phase != cur_phase:
                        cur_phase = o.phase
                        phase_slots = {}
                    if id(b) not in phase_slots:
                        phase_slots[id(b)] = len(phase_slots)
                        if len(pool_cnt) < len(phase_slots):
                            pool_cnt.append(0)
                    sl = phase_slots[id(b)]
                    pool_cnt[sl] += o.dma_inc
                    o.dma_val = pool_cnt[sl]
                    o.dsem = ("Q", sl)
                b.dma_cnt = o.dma_val
                b.dma_sem = o.dsem
            elif o.signal:
                cnt[o.eng] += 1
                o.semval = cnt[o.eng]
        self.stats = dict(cnt=cnt, n_ops=len(ops), n_pers=len(pers_cnt), n_pool=len(pool_cnt))
        with ExitStack() as es:
            esem = {e: es.enter_context(nc.semaphore("s_" + e)) for e in ENGINES if e != "sync"}
            psems = {("P", i): es.enter_context(nc.semaphore("dp%d" % i)) for i in range(len(pers_cnt))}
            psems.update({("Q", i): es.enter_context(nc.semaphore("dq%d" % i)) for i in range(len(pool_cnt))})
            block = es.enter_context(nc.Block())

            def make(engname):
                def body(eng):
                    seen = {}
                    for o in per_eng[engname]:
                        waits = {}
                        for d in o.deps:
                            p = ops[d]
                            if p.is_dma:
                                key = p.dsem
                                sem = psems[p.dsem]
                                val = p.dma_val
                            else:
                                if p.eng == engname and engname == "tensor":
                                    continue
                                key = ("e", p.eng)
                                sem = esem[p.eng]
                                val = p.semval
                            if seen.get(key, 0) >= val:
                                continue
                            if key not in waits or waits[key][1] < val:
                                waits[key] = (sem, val)
                        for key, (sem, val) in waits.items():
                            eng.wait_ge(sem, val)
                            seen[key] = val
                        if o.fn is None:
                            continue
                        ins = o.fn(eng)
                        if o.is_dma:
                            ins.then_inc(psems[o.dsem], o.dma_inc)
                        elif o.signal:
                            ins.then_inc(esem[engname], 1)
                    if engname == "sync":
                        for b in final_wait_bufs:
                            eng.wait_ge(psems[b.dma_sem], b.dma_cnt)
                return body

            for e in ENGINES:
                if per_eng[e] or e == "sync":
                    getattr(block, e)(make(e))


class T:
    __slots__ = ("t", "b")

    def __init__(self, t, b):
        self.t = t
        self.b = b

    def __getitem__(self, k):
        return self.t[k]


class PB:
    def __init__(self):
        self.nc = bass.Bass("TRN2", target_bir_lowering=False)
        self.S = Sched(self.nc)
        self.es = ExitStack()
        self.outs = []
        self._n = 0
        self.bind = {}
        self.fused = False
        self.pes = None
        self._banks = None

    def banks(self):
        if self._banks is None:
            self._banks = []
            for i in range(8):
                t = self.es.enter_context(self.nc.psum_tensor("bank%d" % i, [128, 512], F32))
                self._banks.append(T(t, self.S.buf("bank%d" % i)))
        return self._banks

    def begin(self, bind):
        self.bind = bind
        self.pes = ExitStack()

    def end(self):
        self.S.fence()
        self.pes.close()
        self.pes = None
        self.bind = {}

    def _name(self, n):
        self._n += 1
        return "%s_%d" % (n, self._n)

    def sb(self, name, shape, dt=F32):
        st = self.pes if self.pes is not None else self.es
        t = st.enter_context(self.nc.sbuf_tensor(self._name(name), list(shape), dt))
        return T(t, self.S.buf(name))

    def ps(self, name, shape, dt=F32):
        t = self.es.enter_context(self.nc.psum_tensor(self._name(name), list(shape), dt))
        return T(t, self.S.buf(name))

    def din(self, name, shape, dt=F32):
        if name in self.bind:
            return self.bind[name]
        t = self.nc.dram_tensor(name, list(shape), dt, kind="ExternalInput").ap()
        return T(t, self.S.buf(name))

    def dout(self, name, shape, dt=F32):
        if name in self.bind:
            return self.bind[name]
        t = self.nc.dram_tensor(name, list(shape), dt, kind="ExternalOutput").ap()
        r = T(t, self.S.buf(name))
        r.b.nowaw = True
        r.b.persistent = True
        self.outs.append(r)
        return r

    def dscratch(self, name, shape, dt=F32):
        if name in self.bind:
            return self.bind[name]
        t = self.nc.dram_tensor(self._name(name), list(shape), dt, kind="Internal").ap()
        r = T(t, self.S.buf(name))
        r.b.nowaw = True
        r.b.persistent = True
        return r

    def dma(self, eng, out, in_, r, w, **kw):
        return self.S.op(eng, lambda e: e.dma_start(out=out, in_=in_, **kw), reads=[x.b for x in r],
                         writes=[w.b], dma=True)

    def op(self, eng, fn, r, w):
        return self.S.op(eng, fn, reads=[x.b for x in r], writes=[x.b for x in w])

    def mm(self, out, lhsT, rhs, start, stop, r, w):
        return self.op("tensor", lambda e: e.matmul(out, lhsT=lhsT, rhs=rhs, start=start, stop=stop), r, w)

    def act(self, eng_unused, out, in_, func, r, w, **kw):
        return self.op("scalar", lambda e: e.activation(out=out, in_=in_, func=func, **kw), r, w)

    def finish(self):
        self.S.emit(final_wait_bufs=[o.b for o in self.outs])
        self.es.close()
        return self.nc


def run(pb_nc, in_maps):
    res = run_bass_kernel_spmd(pb_nc, in_maps, core_ids=list(range(len(in_maps))))
    return res.results


def rep128(v):
    v = np.asarray(v, dtype=np.float32).reshape(1, -1)
    return np.ascontiguousarray(np.broadcast_to(v, (128, v.shape[1])))


def make_ident(pb, dt):
    idf = pb.sb("idf", [128, 128], F32)
    pb.op("gpsimd", lambda e: e.memset(idf[:], 1.0), [], [idf])
    pb.op("gpsimd", lambda e: e.affine_select(out=idf[:], in_=idf[:], pattern=[[-1, 128]],
                                                compare_op=ALU.is_equal, fill=0.0, base=0,
                                                channel_multiplier=1), [idf], [idf])
    if dt == F32:
        return idf
    idb = pb.sb("idb", [128, 128], dt)
    pb.op("vector", lambda e: e.tensor_copy(out=idb[:], in_=idf[:]), [idf], [idb])
    return idb


def tri_mask(pb, name, op, dt):
    mf = pb.sb(name + "f", [128, 128], F32)
    pb.op("gpsimd", lambda e: e.memset(mf[:], 1.0), [], [mf])
    if op == ALU.is_le:
        pat, cm = [[1, 128]], -1
    else:
        pat, cm = [[-1, 128]], 1
    pb.op("gpsimd", lambda e: e.affine_select(out=mf[:], in_=mf[:], pattern=pat,
                                                compare_op=ALU.is_ge, fill=0.0, base=0,
                                                channel_multiplier=cm), [mf], [mf])
    if dt == F32:
        return mf
    mb = pb.sb(name, [128, 128], dt)
    pb.op("vector", lambda e: e.tensor_copy(out=mb[:], in_=mf[:]), [mf], [mb])
    return mb


def build_L0():
    pb = PB()
    NCOL = 3072
    cT = pb.din("cT", [128, 16, 4])
    w = pb.din("w", [D, NCOL])
    brep = pb.din("brep", [4, NCOL])
    out = pb.dout("out", [4, NCOL])
    cs = pb.sb("cs", [128, 16, 4])
    cb = pb.sb("cb", [128, 16, 4], BF16)
    bs = pb.sb("bs", [4, NCOL])
    os_ = pb.sb("os", [4, NCOL])
    pb.dma("sync", cs[:], cT[:], [cT], cs)
    pb.dma("sync", bs[:], brep[:], [brep], bs)
    pb.act(None, cb[:], cs[:], AF.Silu, [cs], [cb])
    wv = w.t.rearrange("(k p) n -> p k n", p=128)
    wts = [pb.sb("w%d" % i, [128, 16, 512], BF16) for i in range(2)]
    pss = [pb.ps("ps%d" % i, [4, 512]) for i in range(2)]
    for j in range(6):
        wt = wts[j % 2]
        pst = pss[j % 2]
        pb.dma("gpsimd", wt[:], wv[:, :, j * 512:(j + 1) * 512], [w], wt)
        for k in range(16):
            pb.mm(pst[:], cb[:, k, :], wt[:, k, :], k == 0, k == 15, [cb, wt], [pst])
        pb.op("vector", lambda e, j=j, pst=pst: e.tensor_tensor(out=os_[:, j * 512:(j + 1) * 512], in0=pst[:],
                                                               in1=bs[:, j * 512:(j + 1) * 512], op=ALU.add),
              [pst, bs], [os_])
    pb.dma("sync", out[:], os_[:], [os_], out)
    return pb.finish()


def run_L0(c, w_ada, b_ada):
    nc = build_L0()
    cT = np.ascontiguousarray(c.reshape(4, 16, 128).transpose(2, 1, 0))
    maps = []
    for i in range(8):
        l, q = i // 4, i % 4
        maps.append({"cT": cT, "w": np.ascontiguousarray(w_ada[l][:, q * 3072:(q + 1) * 3072]),
                     "brep": np.ascontiguousarray(np.broadcast_to(b_ada[l][q * 3072:(q + 1) * 3072], (4, 3072)))})
    res = run(nc, maps)
    mod = np.zeros((DEPTH, 4, 6 * D), np.float32)
    for i in range(8):
        l, q = i // 4, i % 4
        mod[l][:, q * 3072:(q + 1) * 3072] = res[i]["out"]
    return mod


def emit_norm_mod(pb, xt, onepsc, sh, junk, ssq, hout):
    pb.act(None, junk[:], xt[:], AF.Square, [xt], [junk, ssq], accum_out=ssq[:])
    pb.act(None, ssq[:], ssq[:], AF.Sqrt, [ssq], [ssq], bias=EPS, scale=1.0 / D)
    pb.op("vector", lambda e: e.reciprocal(out=ssq[:], in_=ssq[:]), [ssq], [ssq])
    pb.op("vector", lambda e: e.scalar_tensor_tensor(out=junk[:], in0=xt[:], scalar=ssq[:, 0:1], in1=onepsc[:],
                                                       op0=ALU.mult, op1=ALU.mult), [xt, ssq, onepsc], [junk])
    pb.op("gpsimd", lambda e: e.tensor_tensor(out=hout[:], in0=junk[:], in1=sh[:], op=ALU.add), [junk, sh], [hout])


def build_L1(prologue, pb=None):
    own = pb is None
    pb = pb or PB()
    NT = 8
    x = pb.din("x", [1024, D])
    sc = pb.din("sc", [128, D])
    shd = pb.din("sh", [128, D])
    w = pb.din("w", [D, INW])
    pre = pb.dout("pre", [1024, INW])
    if prologue:
        p0 = pb.din("p0", [1024, D])
        p1 = pb.din("p1", [1024, D])
        g2d = pb.din("g2", [128, D])
        xo = pb.dout("xo", [1024, D])
        g2 = pb.sb("g2s", [128, D])
        pb.dma("sync", g2[:], g2d[:], [g2d], g2)
    ident = make_ident(pb, BF16)
    onepsc = pb.sb("onepsc", [128, D])
    shs = pb.sb("shs", [128, D])
    pb.dma("sync", onepsc[:], sc[:], [sc], onepsc)
    pb.dma("sync", shs[:], shd[:], [shd], shs)
    pb.op("vector", lambda e: e.tensor_scalar(out=onepsc[:], in0=onepsc[:], scalar1=1.0, scalar2=None, op0=ALU.add),
          [onepsc], [onepsc])
    hT = pb.sb("hT", [128, 16, 1024], BF16)
    xts = [pb.sb("xt%d" % i, [128, D]) for i in range(2)]
    pts = [pb.sb("pt%d" % i, [128, D]) for i in range(2)] if prologue else None
    junk = pb.sb("junk", [128, D])
    hb = pb.sb("hb", [128, D], BF16)
    ssq = pb.sb("ssq", [128, 1])
    ptr = pb.banks()[4:6]
    for t in range(NT):
        xt = xts[t % 2]
        pb.dma("sync", xt[:], x[t * 128:(t + 1) * 128, :], [x], xt)
        if prologue:
            pt = pts[t % 2]
            pb.dma("sync", pt[:], p0[t * 128:(t + 1) * 128, :], [p0], pt)
            pb.dma("sync", junk[:], p1[t * 128:(t + 1) * 128, :], [p1], junk)
            pb.op("vector", lambda e, pt=pt: e.tensor_tensor(out=pt[:], in0=pt[:], in1=junk[:], op=ALU.add), [pt, junk], [pt])
            pb.op("vector", lambda e, pt=pt: e.tensor_tensor(out=pt[:], in0=pt[:], in1=g2[:], op=ALU.mult), [pt, g2], [pt])
            pb.op("vector", lambda e, pt=pt, xt=xt: e.tensor_tensor(out=xt[:], in0=xt[:], in1=pt[:], op=ALU.add), [pt, xt], [xt])
            pb.dma("sync", xo[t * 128:(t + 1) * 128, :], xt[:], [xt], xo)
        emit_norm_mod(pb, xt, onepsc, shs, junk, ssq, hb)
        for g in range(4):
            pt_ = ptr[g % 2]
            for q in range(4):
                k = g * 4 + q
                pb.mm(pt_[:, q * 128:(q + 1) * 128], hb[:, k * 128:(k + 1) * 128], ident[:], True, True, [hb, ident], [pt_])
            pb.act(None, hT[:, g * 4:(g + 1) * 4, t * 128:(t + 1) * 128],
                   pt_[:].rearrange("p (q n) -> p q n", q=4), AF.Copy, [pt_], [hT])
    wv = w.t.rearrange("(k p) n -> p k n", p=128)
    wts = [pb.sb("w%d" % i, [128, 16, 512], BF16) for i in range(2)]
    pss = pb.banks()[0:4]
    stg = [pb.sb("stg%d" % i, [128, 512]) for i in range(4)]
    cnt = 0
    for j in range(10):
        c0 = j * 512
        cw = min(512, INW - c0)
        wt = wts[j % 2]
        pb.dma("gpsimd", wt[:, :, :cw], wv[:, :, c0:c0 + cw], [w], wt)
        for t in range(NT):
            pst = pss[cnt % 4]
            st = stg[cnt % 4]
            for k in range(16):
                pb.mm(pst[:, :cw], hT[:, k, t * 128:(t + 1) * 128], wt[:, k, :cw], k == 0, k == 15, [hT, wt], [pst])
            if cnt % 2 == 0:
                pb.op("vector", lambda e, st=st, pst=pst, cw=cw: e.tensor_copy(out=st[:, :cw], in_=pst[:, :cw]), [pst], [st])
            else:
                pb.act(None, st[:, :cw], pst[:, :cw], AF.Copy, [pst], [st])
            pb.dma("sync", pre[t * 128:(t + 1) * 128, c0:c0 + cw], st[:, :cw], [st], pre)
            cnt += 1
    return pb.finish() if own else None


def run_L1(nc, xs, mod_l, w_in_l, prologue=None):
    maps = []
    for i in range(8):
        b, r = i // 2, i % 2
        sl = slice(r * 1024, (r + 1) * 1024)
        m = {"x": np.ascontiguousarray(xs[b, sl]), "sc": rep128(mod_l[b, D:2 * D]), "sh": rep128(mod_l[b, 0:D]),
             "w": w_in_l}
        if prologue is not None:
            pp, g2 = prologue
            m["p0"] = np.ascontiguousarray(pp[2 * b][sl])
            m["p1"] = np.ascontiguousarray(pp[2 * b + 1][sl])
            m["g2"] = rep128(g2[b])
        maps.append(m)
    res = run(nc, maps)
    pre = np.zeros((4, S, INW), np.float32)
    xo = np.zeros((4, S, D), np.float32) if prologue is not None else None
    for i in range(8):
        b, r = i // 2, i % 2
        pre[b, r * 1024:(r + 1) * 1024] = res[i]["pre"]
        if prologue is not None:
            xo[b, r * 1024:(r + 1) * 1024] = res[i]["xo"]
    return pre, xo


def build_L2(pb=None):
    own = pb is None
    pb = pb or PB()
    NT = 16
    if "aq" in pb.bind:
        aqd, akd = pb.bind["aq"], pb.bind["ak"]
    else:
        qk = pb.din("qk", [S, 640])
        aqd, akd = T(qk.t[:, 0:512], qk.b), T(qk.t[:, 512:640], qk.b)
    csd = pb.din("cs", [S, 640])
    snd = pb.din("sn", [S, 640])
    gaind = pb.din("gain", [128, 640])
    avd = pb.din("av", [S, 128])
    sinkd = pb.din("sinkr", [128, 512])
    mqd = pb.din("mq", [S, 256])
    mkd = pb.din("mk", [S, 256])
    mvd = pb.din("mv", [S, 512])
    mod_ = pb.din("mo", [S, 512])
    gtd = pb.din("gt", [S, 8]) if "gt16" not in pb.bind else None
    bgd = pb.din("bg", [128, 128])
    mgd = pb.din("mgain", [128, 512])
    attT = pb.dout("attT", [4, 128, S], BF16)
    moT = pb.dout("moT", [4, 128, S], BF16)

    PS = pb.banks()
    ident = make_ident(pb, BF16)
    m_ge_f = tri_mask(pb, "mge", ALU.is_ge, F32)
    m_le_f = tri_mask(pb, "mle", ALU.is_le, F32)
    m_ge = pb.sb("mgeb", [128, 128], BF16)
    m_le = pb.sb("mleb", [128, 128], BF16)
    pb.op("vector", lambda e: e.tensor_copy(out=m_ge[:], in_=m_ge_f[:]), [m_ge_f], [m_ge])
    pb.op("vector", lambda e: e.tensor_copy(out=m_le[:], in_=m_le_f[:]), [m_le_f], [m_le])
    m_ge4 = pb.sb("mge4", [128, 4, 128], BF16)
    m_le4 = pb.sb("mle4", [128, 4, 128], BF16)
    for g in range(4):
        pb.op("vector", lambda e, g=g: e.tensor_copy(out=m_ge4[:, g, :], in_=m_ge_f[:]), [m_ge_f], [m_ge4])
        pb.op("vector", lambda e, g=g: e.tensor_copy(out=m_le4[:, g, :], in_=m_le_f[:]), [m_le_f], [m_le4])
    ones_f = pb.sb("ones_f", [128, 128])
    ones_b = pb.sb("ones_b", [128, 128], BF16)
    pb.op("gpsimd", lambda e: e.memset(ones_f[:], 1.0), [], [ones_f])
    pb.op("gpsimd", lambda e: e.memset(ones_b[:], 1.0), [], [ones_b])

    gain = pb.sb("gain", [128, 640])
    pb.dma("sync", gain[:], gaind[:], [gaind], gain)
    sinke = pb.sb("sinke", [128, 512])
    pb.dma("sync", sinke[:], sinkd[:], [sinkd], sinke)
    pb.act(None, sinke[:], sinke[:], AF.Exp, [sinke], [sinke])

    qT = pb.sb("qT", [128, NT, 512], BF16)
    kT = pb.sb("kT", [128, NT, 128], BF16)
    vb = pb.sb("vb", [128, NT, 128], BF16)
    attTs = pb.sb("attTs", [128, 4, S], BF16)

    qkt = [pb.sb("qkt%d" % i, [128, 640]) for i in range(2)]
    cst = [pb.sb("cst%d" % i, [128, 640]) for i in range(2)]
    snt = [pb.sb("snt%d" % i, [128, 640]) for i in range(2)]
    avt = [pb.sb("avt%d" % i, [128, 128]) for i in range(2)]
    junk = pb.sb("junk5", [128, 640])
    qn = pb.sb("qn", [128, 640])
    ra = pb.sb("ra", [128, 640])
    rb = pb.sb("rb", [128, 640])
    rot = pb.sb("rot", [128, 640], BF16)
    ss5 = pb.sb("ss5", [128, 5])
    for t in range(NT):
        x_ = qkt[t % 2]
        c_ = cst[t % 2]
        s_ = snt[t % 2]
        a_ = avt[t % 2]
        rs = slice(t * 128, (t + 1) * 128)
        pb.dma("sync", x_[:, 0:512], aqd[rs, :], [aqd], x_)
        pb.dma("sync", x_[:, 512:640], akd[rs, :], [akd], x_)
        pb.dma("sync", c_[:], csd[rs, :], [csd], c_)
        pb.dma("sync", s_[:], snd[rs, :], [snd], s_)
        pb.dma("sync", a_[:], avd[rs, :], [avd], a_)
        pb.act(None, junk[:], x_[:], AF.Square, [x_], [junk])
        pb.op("vector", lambda e: e.tensor_reduce(out=ss5[:], in_=junk[:].rearrange("p (h d) -> p h d", h=5),
                                                   axis=AX.X, op=ALU.add), [junk], [ss5])
        pb.act(None, ss5[:], ss5[:], AF.Sqrt, [ss5], [ss5], bias=EPS, scale=1.0 / 128)
        pb.op("vector", lambda e: e.reciprocal(out=ss5[:], in_=ss5[:]), [ss5], [ss5])
        for h in range(5):
            hs = slice(h * 128, (h + 1) * 128)
            pb.op("vector", lambda e, hs=hs, h=h, x_=x_: e.scalar_tensor_tensor(
                out=qn[:, hs], in0=x_[:, hs], scalar=ss5[:, h:h + 1], in1=gain[:, hs], op0=ALU.mult, op1=ALU.mult),
                [x_, ss5, gain], [qn])
        pb.op("gpsimd", lambda e, c_=c_: e.tensor_tensor(out=ra[:], in0=qn[:], in1=c_[:], op=ALU.mult), [qn, c_], [ra])
        qn4 = qn[:].rearrange("p (h t d) -> p h t d", h=5, t=2)
        rb4 = rb[:].rearrange("p (h t d) -> p h t d", h=5, t=2)
        sn4 = s_[:].rearrange("p (h t d) -> p h t d", h=5, t=2)
        pb.op("vector", lambda e, qn4=qn4, rb4=rb4, sn4=sn4: e.tensor_tensor(
            out=rb4[:, :, 0, :], in0=qn4[:, :, 1, :], in1=sn4[:, :, 0, :], op=ALU.mult), [qn, s_], [rb])
        pb.op("vector", lambda e, qn4=qn4, rb4=rb4, sn4=sn4: e.tensor_tensor(
            out=rb4[:, :, 1, :], in0=qn4[:, :, 0, :], in1=sn4[:, :, 1, :], op=ALU.mult), [qn, s_], [rb])
        pb.op("gpsimd", lambda e: e.tensor_tensor(out=rot[:], in0=ra[:], in1=rb[:], op=ALU.add), [ra, rb], [rot])
        pq = PS[t % 2]
        pk = PS[2 + t % 2]
        for h in range(4):
            pb.mm(pq[:, h * 128:(h + 1) * 128], rot[:, h * 128:(h + 1) * 128], ident[:], True, True, [rot, ident], [pq])
        pb.mm(pk[:, 0:128], rot[:, 512:640], ident[:], True, True, [rot, ident], [pk])
        pb.act(None, qT[:, t, :], pq[:], AF.Copy, [pq], [qT])
        pb.op("vector", lambda e, pk=pk, t=t: e.tensor_copy(out=kT[:, t, :], in_=pk[:, 0:128]), [pk], [kT])
        pb.op("gpsimd", lambda e, a_=a_, t=t: e.tensor_copy(out=vb[:, t, :], in_=a_[:]), [a_], [vb])

    Es = [pb.sb("E%d" % i, [128, 512], BF16) for i in range(4)]
    dens = [pb.sb("den%d" % i, [128, 512]) for i in range(2)]
    ec = 0
    for n in range(NT):
        po = PS[4 + n % 2]
        pd = PS[6 + n % 2]
        kbs = [kb for kb in (n - 1, n, n + 1) if 0 <= kb < NT]
        for i, kb in enumerate(kbs):
            psc = PS[ec % 4]
            E = Es[ec % 4]
            ec += 1
            pb.mm(psc[:], kT[:, kb, :], qT[:, n, :], True, True, [kT, qT], [psc])
            pb.act(None, E[:], psc[:], AF.Exp, [psc], [E], scale=1.0 / math.sqrt(128.0))
            if kb != n:
                mk4 = m_ge4 if kb < n else m_le4
                pb.op("gpsimd", lambda e, E=E, mk4=mk4: e.tensor_tensor(
                    out=E[:], in0=E[:], in1=mk4[:].rearrange("p g q -> p (g q)"), op=ALU.mult), [E, mk4], [E])
            pb.mm(po[:], vb[:, kb, :], E[:], i == 0, i == len(kbs) - 1, [vb, E], [po])
            pb.mm(pd[:], ones_b[:], E[:], i == 0, i == len(kbs) - 1, [ones_b, E], [pd])
        dn = dens[n % 2]
        pb.op("vector", lambda e, dn=dn, pd=pd: e.tensor_tensor(out=dn[:], in0=pd[:], in1=sinke[:], op=ALU.add), [pd, sinke], [dn])
        pb.op("vector", lambda e, dn=dn: e.reciprocal(out=dn[:], in_=dn[:]), [dn], [dn])
        pb.op("vector", lambda e, dn=dn, po=po, n=n: e.tensor_tensor(
            out=attTs[:, :, n * 128:(n + 1) * 128], in0=po[:].rearrange("p (g q) -> p g q", g=4),
            in1=dn[:].rearrange("p (g q) -> p g q", g=4), op=ALU.mult), [po, dn], [attTs])
    for g in range(4):
        pb.dma("sync", attT[g], attTs[:, g, :], [attTs], attT)

    mq = pb.sb("mq", [128, NT, 256])
    mk = pb.sb("mk", [128, NT, 256])
    v1 = pb.sb("v1", [128, NT, 2, 257], BF16)
    hacc = pb.sb("hacc", [128, NT, 512])
    gt = pb.sb("gt", [128, NT, 8])
    bg = pb.sb("bg", [128, NT, 8])
    pb.dma("sync", mq[:], mqd.t.rearrange("(t p) c -> p t c", p=128), [mqd], mq)
    pb.dma("sync", mk[:], mkd.t.rearrange("(t p) c -> p t c", p=128), [mkd], mk)
    if gtd is not None:
        pb.dma("sync", gt[:], gtd.t.rearrange("(t p) c -> p t c", p=128), [gtd], gt)
    else:
        g16d, rsel = pb.bind["gt16"], pb.bind["rsel"]
        gt16 = pb.sb("gt16", [128, NT, 16])
        pb.dma("sync", gt16[:], g16d.t.rearrange("(t p) c -> p t c", p=128), [g16d], gt16)
        pb.op("vector", lambda e: e.tensor_copy(
            out=gt[:].rearrange("p t (g h) -> p t g h", g=4),
            in_=gt16[:].rearrange("p t (g h) -> p t g h", g=4)[:, :, :, 2 * rsel:2 * rsel + 2]), [gt16], [gt])
    pb.dma("sync", bg[:], bgd.t.rearrange("p (t c) -> p t c", c=8), [bgd], bg)
    pb.op("gpsimd", lambda e: e.memset(v1[:, :, :, 256:257], 1.0), [], [v1])
    mvt = [pb.sb("mvt%d" % i, [128, 512]) for i in range(2)]
    for t in range(NT):
        m_ = mvt[t % 2]
        pb.dma("sync", m_[:], mvd[t * 128:(t + 1) * 128, :], [mvd], m_)
        pb.op("gpsimd", lambda e, m_=m_, t=t: e.tensor_copy(out=v1[:, t, :, 0:256],
                                                             in_=m_[:].rearrange("p (h c) -> p h c", h=2)), [m_], [v1])
    pb.op("vector", lambda e: e.tensor_tensor(out=gt[:], in0=gt[:], in1=bg[:], op=ALU.add), [gt, bg], [gt])
    logf = pb.sb("logf", [128, NT, 4])
    pb.act(None, logf[:], gt[:, :, 4:8], AF.Exp, [gt], [logf], scale=-1.0)
    pb.act(None, logf[:], logf[:], AF.Ln, [logf], [logf], bias=1.0, scale=1.0)
    pb.op("vector", lambda e: e.tensor_scalar(out=logf[:], in0=logf[:], scalar1=-1.0, scalar2=None, op0=ALU.mult), [logf], [logf])
    pc = PS[0]
    pcv = pc[:, 0:128].rearrange("p (t c) -> p t c", c=8)
    for t in range(NT):
        pb.mm(pcv[:, t, 0:2], m_le_f[:], logf[:, t, 0:2], True, True, [m_le_f, logf], [pc])
        pb.mm(pcv[:, t, 2:4], m_ge_f[:], logf[:, t, 2:4], True, True, [m_ge_f, logf], [pc])
        pb.mm(pcv[:, t, 4:8], ones_f[:], logf[:, t, 0:4], True, True, [ones_f, logf], [pc])
    args = pb.sb("args", [128, NT, 16])
    pb.op("vector", lambda e: e.tensor_copy(out=args[:, :, 0:4], in_=pcv[:, :, 0:4]), [pc], [args])
    pb.op("vector", lambda e: e.tensor_tensor(out=args[:, :, 4:8], in0=gt[:, :, 0:4], in1=args[:, :, 0:4], op=ALU.subtract), [gt, args], [args])
    pb.op("vector", lambda e: e.tensor_tensor(out=args[:, :, 8:12], in0=args[:, :, 4:8], in1=pcv[:, :, 4:8], op=ALU.add), [pc, args], [args])
    pb.op("vector", lambda e: e.tensor_copy(out=args[:, :, 12:16], in_=pcv[:, :, 4:8]), [pc], [args])
    E16 = pb.sb("E16", [128, NT, 16])
    pb.act(None, E16[:], args[:], AF.Exp, [args], [E16])

    Sst = [pb.sb("Sst%d" % i, [128, 257]) for i in range(2)]
    Sbf = [pb.sb("Sbf%d" % i, [128, 257], BF16) for i in range(2)]
    NBUF = 1
    qs = [pb.sb("qs%d" % i, [128, 128], BF16) for i in range(2 * NBUF)]
    ks = [pb.sb("ks%d" % i, [128, 128], BF16) for i in range(2 * NBUF)]
    kst = [pb.sb("kst%d" % i, [128, 128], BF16) for i in range(2 * NBUF)]
    qkTs = [pb.sb("qkT%d" % i, [128, 256], BF16) for i in range(2 * NBUF)]
    smT = [pb.sb("smT%d" % i, [128, 128], BF16) for i in range(2 * NBUF)]
    dnm = [pb.sb("dnm%d" % i, [128, 1]) for i in range(2 * NBUF)]
    DK = 128.0 ** -0.5
    for dr in range(2):
        msk = m_le_f if dr == 0 else m_ge_f
        for hh in range(2):
            pb.op("gpsimd", lambda e, hh=hh: e.memset(Sst[hh][:], 0.0), [], [Sst[hh]])
            pb.op("gpsimd", lambda e, hh=hh: e.memset(Sbf[hh][:], 0.0), [], [Sbf[hh]])
        for ci in range(NT):
            t = ci if dr == 0 else NT - 1 - ci
            for hh in range(2):
                col = dr * 2 + hh
                hs = slice(hh * 128, (hh + 1) * 128)
                bi = hh * NBUF + ci % NBUF
                q_, k_, ks_, qkT_, sm_, dn_ = qs[bi], ks[bi], kst[bi], qkTs[bi], smT[bi], dnm[bi]
                ptr = PS[hh * 4 + 0]
                psm = PS[hh * 4 + 0]
                pso = PS[hh * 4 + 1 + ci % 2]
                pkv = PS[hh * 4 + 3]
                pb.op("vector", lambda e, q_=q_, t=t, hs=hs, col=col: e.tensor_scalar(
                    out=q_[:], in0=mq[:, t, hs], scalar1=E16[:, t, col:col + 1], scalar2=DK, op0=ALU.mult, op1=ALU.mult),
                    [mq, E16], [q_])
                pb.act(None, k_[:], mk[:, t, hs], AF.Copy, [mk, E16], [k_], scale=E16[:, t, 4 + col:5 + col])
                pb.act(None, ks_[:], mk[:, t, hs], AF.Copy, [mk, E16], [ks_], scale=E16[:, t, 8 + col:9 + col])
                pb.mm(ptr[:, 0:128], q_[:], ident[:], True, True, [q_, ident], [ptr])
                pb.mm(ptr[:, 128:256], k_[:], ident[:], True, True, [k_, ident], [ptr])
                pb.op("vector", lambda e, qkT_=qkT_, ptr=ptr: e.tensor_copy(out=qkT_[:], in_=ptr[:, 0:256]), [ptr], [qkT_])
                pb.mm(psm[:, 256:384], qkT_[:, 128:256], qkT_[:, 0:128], True, True, [qkT_], [psm])
                pb.op("vector", lambda e, sm_=sm_, psm=psm, msk=msk: e.tensor_tensor(
                    out=sm_[:], in0=psm[:, 256:384], in1=msk[:], op=ALU.mult), [psm, msk], [sm_])
                pb.mm(pso[:, 0:257], sm_[:], v1[:, t, hh, :], True, False, [sm_, v1], [pso])
                pb.mm(pso[:, 0:257], qkT_[:, 0:128], Sbf[hh][:], False, True, [qkT_, Sbf[hh]], [pso])
                pb.act(None, dn_[:], pso[:, 256:257], AF.Abs, [pso], [dn_])
                pb.op("vector", lambda e, dn_=dn_: e.tensor_scalar(
                    out=dn_[:], in0=dn_[:], scalar1=1.0, scalar2=None, op0=ALU.max), [dn_], [dn_])
                pb.op("vector", lambda e, dn_=dn_: e.reciprocal(out=dn_[:], in_=dn_[:]), [dn_], [dn_])
                hsl = slice(hh * 256, (hh + 1) * 256)
                if dr == 0:
                    pb.op("vector", lambda e, dn_=dn_, pso=pso, t=t, hsl=hsl: e.tensor_scalar(
                        out=hacc[:, t, hsl], in0=pso[:, 0:256], scalar1=dn_[:, 0:1], scalar2=None, op0=ALU.mult),
                        [pso, dn_], [hacc])
                else:
                    pb.op("vector", lambda e, dn_=dn_, pso=pso, t=t, hsl=hsl: e.scalar_tensor_tensor(
                        out=hacc[:, t, hsl], in0=pso[:, 0:256], scalar=dn_[:, 0:1], in1=hacc[:, t, hsl],
                        op0=ALU.mult, op1=ALU.add), [pso, dn_, hacc], [hacc])
                pb.mm(pkv[:, 0:257], ks_[:], v1[:, t, hh, :], True, True, [ks_, v1], [pkv])
                pb.op("vector", lambda e, hh=hh, pkv=pkv, t=t, col=col: e.scalar_tensor_tensor(
                    out=Sst[hh][:], in0=Sst[hh][:], scalar=E16[:, t, 12 + col:13 + col], in1=pkv[:, 0:257],
                    op0=ALU.mult, op1=ALU.add), [Sst[hh], E16, pkv], [Sst[hh]])
                pb.act(None, Sbf[hh][:], Sst[hh][:], AF.Copy, [Sst[hh]], [Sbf[hh]])

    mgain = pb.sb("mgain", [128, 512])
    pb.dma("sync", mgain[:], mgd[:], [mgd], mgain)
    moTs = pb.sb("moTs", [128, 4, S], BF16)
    mot = [pb.sb("mot%d" % i, [128, 512]) for i in range(2)]
    hn = pb.sb("hn", [128, 512])
    mob = pb.sb("mob", [128, 512], BF16)
    jk = pb.sb("jk", [128, 256])
    ss2 = pb.sb("ss2", [128, 2])
    for t in range(NT):
        o_ = mot[t % 2]
        pb.dma("sync", o_[:], mod_[t * 128:(t + 1) * 128, :], [mod_], o_)
        pb.act(None, o_[:], o_[:], AF.Sigmoid, [o_], [o_])
        for hh in range(2):
            pb.act(None, jk[:], hacc[:, t, hh * 256:(hh + 1) * 256], AF.Square, [hacc], [jk, ss2], accum_out=ss2[:, hh:hh + 1])
        pb.act(None, ss2[:], ss2[:], AF.Sqrt, [ss2], [ss2], bias=EPS, scale=1.0 / 256)
        pb.op("vector", lambda e: e.reciprocal(out=ss2[:], in_=ss2[:]), [ss2], [ss2])
        for hh in range(2):
            hsl = slice(hh * 256, (hh + 1) * 256)
            pb.op("vector", lambda e, hh=hh, hsl=hsl, t=t: e.scalar_tensor_tensor(
                out=hn[:, hsl], in0=hacc[:, t, hsl], scalar=ss2[:, hh:hh + 1], in1=mgain[:, hsl],
                op0=ALU.mult, op1=ALU.mult), [hacc, ss2, mgain], [hn])
        pb.op("gpsimd", lambda e, o_=o_: e.tensor_tensor(out=mob[:], in0=hn[:], in1=o_[:], op=ALU.mult), [hn, o_], [mob])
        pp = PS[t % 2]
        for j in range(4):
            pb.mm(pp[:, j * 128:(j + 1) * 128], mob[:, j * 128:(j + 1) * 128], ident[:], True, True, [mob, ident], [pp])
        pb.act(None, moTs[:, :, t * 128:(t + 1) * 128], pp[:].rearrange("p (j q) -> p j q", j=4), AF.Copy, [pp], [moTs])
    for j in range(4):
        pb.dma("sync", moT[j], moTs[:, j, :], [moTs], moT)
    return pb.finish() if own else None


def rope_tables_np():
    inv = 1.0 / (10000.0 ** (np.arange(0, 128, 2, dtype=np.float32) / 128))
    ang = np.arange(S, dtype=np.float32)[:, None] * inv[None, :]
    ang = np.concatenate([ang, ang], axis=-1)
    cos = np.cos(ang).astype(np.float32)
    sin = np.sin(ang).astype(np.float32)
    sinm = np.concatenate([-sin[:, :64], sin[:, 64:]], axis=-1)
    return np.ascontiguousarray(np.tile(cos, (1, 5))), np.ascontiguousarray(np.tile(sinm, (1, 5)))


def run_L2(nc, pre, b_gates_l, q_gain_l, k_gain_l, sink_l, m_gain_l):
    cs5, sn5 = rope_tables_np()
    maps = []
    for i in range(8):
        b, r = i // 2, i % 2
        p = pre[b]
        aq = p[:, r * 512:(r + 1) * 512]
        ak = p[:, 1024 + r * 128:1024 + (r + 1) * 128]
        av = p[:, 1280 + r * 128:1280 + (r + 1) * 128]
        mq = p[:, 1536 + r * 256:1536 + (r + 1) * 256]
        mk = p[:, 2048 + r * 256:2048 + (r + 1) * 256]
        mv = p[:, 2560 + r * 512:2560 + (r + 1) * 512]
        mo = p[:, 3584 + r * 512:3584 + (r + 1) * 512]
        mg = p[:, 4608:4624].reshape(S, 4, 4)[:, :, 2 * r:2 * r + 2].reshape(S, 8)
        bg = b_gates_l[:, 2 * r:2 * r + 2].reshape(8)
        maps.append({
            "qk": np.ascontiguousarray(np.concatenate([aq, ak], axis=1)),
            "cs": cs5, "sn": sn5,
            "gain": rep128(np.concatenate([np.tile(q_gain_l, 4), k_gain_l])),
            "av": np.ascontiguousarray(av),
            "sinkr": rep128(np.repeat(sink_l[4 * r:4 * r + 4], 128)),
            "mq": np.ascontiguousarray(mq), "mk": np.ascontiguousarray(mk), "mv": np.ascontiguousarray(mv),
            "mo": np.ascontiguousarray(mo), "gt": np.ascontiguousarray(mg),
            "bg": rep128(np.tile(bg, 16)),
            "mgain": rep128(m_gain_l[r * 512:(r + 1) * 512]),
        })
    res = run(nc, maps)
    mixT = np.zeros((4, 16, 128, S), NPBF)
    for i in range(8):
        b, r = i // 2, i % 2
        mixT[b, 4 * r:4 * r + 4] = res[i]["attT"]
        mixT[b, 8 + 4 * r:8 + 4 * r + 4] = res[i]["moT"]
    return mixT


def build_L3(pb=None):
    own = pb is None
    pb = pb or PB()
    NT = 8
    mixd = pb.din("mixT", [16, 128, 1024], BF16) if "mixg" not in pb.bind else None
    x = pb.din("x", [1024, D])
    w = pb.din("w", [D, D])
    g1d = pb.din("g1", [128, D])
    scd = pb.din("sc", [128, D])
    shd = pb.din("sh", [128, D])
    wrd = pb.din("wr", [D, NE])
    x1o = pb.dout("x1", [1024, D])
    h2o = pb.dout("h2", [1024, D], BF16)
    affo = pb.dout("aff", [1024, NE])
    affTo = pb.dout("affT", [NE, 1024])
    PS = pb.banks()
    identf = make_ident(pb, F32)
    mixT = pb.sb("mixT", [128, 16, 1024], BF16)
    if "mixg" in pb.bind:
        mixg, selv = pb.bind["mixg"], pb.bind["selv"]
        sel = pb.sb("sel", [128, 2])
        pb.dma("sync", sel[:], selv[:], [selv], sel)
        mtmp = [pb.sb("mtmp%d" % i, [128, 4, S], BF16) for i in range(1)]
        for qc in range(4):
            mt = mtmp[0]
            q, c0 = qc // 2, (qc % 2) * 4
            ks = slice(qc * 4, qc * 4 + 4)
            pb.dma("sync", mt[:], mixg[qc % 2].t[q].rearrange("c p t -> p c t"), [mixg[qc % 2]], mt)
            pb.op("vector", lambda e, ks=ks, mt=mt: e.tensor_scalar(
                out=mixT[:, ks, :], in0=mt[:, :, 0:1024], scalar1=sel[:, 0:1], scalar2=None, op0=ALU.mult),
                [mt, sel], [mixT])
            pb.op("vector", lambda e, ks=ks, mt=mt: e.scalar_tensor_tensor(
                out=mixT[:, ks, :], in0=mt[:, :, 1024:2048], scalar=sel[:, 1:2],
                in1=mixT[:, ks, :], op0=ALU.mult, op1=ALU.add), [mt, sel, mixT], [mixT])
        wbs = [pb.sb("wbk%d" % i, [128, 1, D], BF16) for i in range(16)]
        for kk in range(16):
            q, c8 = kk // 8, kk % 8
            g = 4 * q + c8 if c8 < 4 else 8 + 4 * q + (c8 - 4)
            pb.dma("gpsimd", wbs[kk][:, 0, :], w[g * 128:(g + 1) * 128, :], [w], wbs[kk])
        wsel = lambda k: wbs[k]
        widx = lambda k: 0
    else:
        wb = pb.sb("wb", [128, 16, D], BF16)
        for c in range(16):
            pb.dma("sync", mixT[:, c, :], mixd[c], [mixd], mixT)
        wv = w.t.rearrange("(k p) n -> p k n", p=128)
        for j in range(4):
            pb.dma("gpsimd", wb[:, :, j * 512:(j + 1) * 512], wv[:, :, j * 512:(j + 1) * 512], [w], wb)
        wsel = lambda k: wb
        widx = lambda k: k
    g1 = pb.sb("g1", [128, D])
    onepsc = pb.sb("onepsc", [128, D])
    shs = pb.sb("shs", [128, D])
    wr = pb.sb("wr", [128, 16, NE])
    pb.dma("sync", g1[:], g1d[:], [g1d], g1)
    pb.dma("sync", onepsc[:], scd[:], [scd], onepsc)
    pb.dma("sync", shs[:], shd[:], [shd], shs)
    pb.dma("sync", wr[:], wrd.t.rearrange("(k p) e -> p k e", p=128), [wrd], wr)
    pb.op("vector", lambda e: e.tensor_scalar(out=onepsc[:], in0=onepsc[:], scalar1=1.0, scalar2=None, op0=ALU.add),
          [onepsc], [onepsc])
    xts = [pb.sb("xt%d" % i, [128, D]) for i in range(2)]
    junk = pb.sb("junk", [128, D])
    h2f = pb.sb("h2f", [128, D])
    h2b = [pb.sb("h2b%d" % i, [128, D], BF16) for i in range(2)]
    h2T = pb.sb("h2T", [128, 16, 128])
    ssq = pb.sb("ssq", [128, 1])
    lg = pb.sb("lg", [128, NE])
    mx = pb.sb("mx", [128, 1])
    sm = pb.sb("sm", [128, 1])
    affs = [pb.sb("affs%d" % i, [128, NE]) for i in range(2)]
    affTs = pb.sb("affTs", [NE, 1024])
    for t in range(NT):
        xt = xts[t % 2]
        ts_ = slice(t * 128, (t + 1) * 128)
        pb.dma("sync", xt[:], x[ts_, :], [x], xt)
        for j in range(4):
            pst = PS[j]
            for k in range(16):
                pb.mm(pst[:], mixT[:, k, ts_], wsel(k)[:, widx(k), j * 512:(j + 1) * 512], k == 0, k == 15, [mixT, wsel(k)], [pst])
            js = slice(j * 512, (j + 1) * 512)
            pb.op("vector", lambda e, pst=pst, js=js: e.tensor_tensor(out=junk[:, js], in0=pst[:], in1=g1[:, js], op=ALU.mult),
                  [pst, g1], [junk])
        pb.op("gpsimd", lambda e, xt=xt: e.tensor_tensor(out=xt[:], in0=xt[:], in1=junk[:], op=ALU.add), [xt, junk], [xt])
        pb.dma("sync", x1o[ts_, :], xt[:], [xt], x1o)
        emit_norm_mod(pb, xt, onepsc, shs, junk, ssq, h2f)
        hb = h2b[t % 2]
        pb.act(None, hb[:], h2f[:], AF.Copy, [h2f], [hb])
        pb.dma("sync", h2o[ts_, :], hb[:], [hb], h2o)
        for g in range(4):
            pt_ = PS[4 + g % 2]
            for q in range(4):
                k = g * 4 + q
                pb.mm(pt_[:, q * 128:(q + 1) * 128], h2f[:, k * 128:(k + 1) * 128], identf[:], True, True, [h2f, identf], [pt_])
            pb.op("vector", lambda e, pt_=pt_, g=g: e.tensor_copy(out=h2T[:, g * 4:(g + 1) * 4, :],
                                                                 in_=pt_[:].rearrange("p (q n) -> p q n", q=4)), [pt_], [h2T])
        pl = PS[6]
        for k in range(16):
            pb.mm(pl[:, 0:NE], h2T[:, k, :], wr[:, k, :], k == 0, k == 15, [h2T, wr], [pl])
        pb.op("vector", lambda e, pl=pl: e.tensor_copy(out=lg[:], in_=pl[:, 0:NE]), [pl], [lg])
        pb.op("vector", lambda e: e.tensor_reduce(out=mx[:], in_=lg[:], axis=AX.X, op=ALU.max), [lg], [mx])
        pb.op("vector", lambda e: e.tensor_scalar(out=mx[:], in0=mx[:], scalar1=-1.0, scalar2=None, op0=ALU.mult), [mx], [mx])
        af = affs[t % 2]
        pb.act(None, af[:], lg[:], AF.Exp, [lg, mx], [af, sm], bias=mx[:, 0:1], scale=1.0, accum_out=sm[:])
        pb.op("vector", lambda e: e.reciprocal(out=sm[:], in_=sm[:]), [sm], [sm])
        pb.op("vector", lambda e, af=af: e.tensor_scalar(out=af[:], in0=af[:], scalar1=sm[:, 0:1], scalar2=None, op0=ALU.mult),
              [af, sm], [af])
        pb.dma("sync", affo[ts_, :], af[:], [af], affo)
        pb.mm(PS[7][0:NE, 0:128], af[:], identf[:], True, True, [af, identf], [PS[7]])
        pb.op("vector", lambda e, ts_=ts_: e.tensor_copy(out=affTs[:, ts_], in_=PS[7][0:NE, 0:128]), [PS[7]], [affTs])
    pb.dma("sync", affTo[:], affTs[:], [affTs], affTo)
    return pb.finish() if own else None


def run_L3(nc, mixT, xs, mod_l, w_out_l, w_router_l):
    maps = []
    for i in range(8):
        b, r = i // 2, i % 2
        sl = slice(r * 1024, (r + 1) * 1024)
        maps.append({"mixT": np.ascontiguousarray(mixT[b][:, :, sl]), "x": np.ascontiguousarray(xs[b, sl]),
                     "w": w_out_l, "g1": rep128(mod_l[b, 2 * D:3 * D]), "sc": rep128(mod_l[b, 4 * D:5 * D]),
                     "sh": rep128(mod_l[b, 3 * D:4 * D]), "wr": w_router_l})
    res = run(nc, maps)
    x1 = np.zeros((4, S, D), np.float32)
    h2 = np.zeros((4, S, D), NPBF)
    aff = np.zeros((4, S, NE), np.float32)
    for i in range(8):
        b, r = i // 2, i % 2
        sl = slice(r * 1024, (r + 1) * 1024)
        x1[b, sl] = res[i]["x1"]
        h2[b, sl] = res[i]["h2"]
        aff[b, sl] = res[i]["aff"]
    return x1, h2, aff


def build_L4(pb=None):
    own = pb is None
    pb = pb or PB()
    NJ = 16
    NEL = 8
    h2d = pb.din("h2", [S, D], BF16) if "h2p" not in pb.bind else None
    affTd = pb.din("affT", [NEL, S])
    afftd = pb.din("afft", [S, NEL])
    wgd = pb.din("wg", [NEL, D, FF])
    wud = pb.din("wu", [NEL, D, FF])
    wdd = pb.din("wd", [NEL, FF, D])
    part = pb.dout("part", [S, D])
    yscr = pb.dscratch("yscr", [NEL * 2, 128, D], BF16)
    yscr.b.nowaw = True
    PS = pb.banks()
    identf = make_ident(pb, F32)
    identb = pb.sb("identb", [128, 128], BF16) if "rstate" not in pb.bind else pb.bind["rstate"]["identb"]
    pb.op("vector", lambda e: e.tensor_copy(out=identb[:], in_=identf[:]), [identf], [identb])
    ones_f = pb.sb("ones_f", [128, 128])
    ones_b = pb.sb("ones_b", [128, 128], BF16)
    pb.op("gpsimd", lambda e: e.memset(ones_f[:], 1.0), [], [ones_f])
    pb.op("gpsimd", lambda e: e.memset(ones_b[:], 1.0), [], [ones_b])
    usf = pb.sb("usf", [128, 128])
    usb = pb.sb("usb", [128, 128], BF16)
    pb.op("gpsimd", lambda e: e.memset(usf[:], 1.0), [], [usf])
    pb.op("gpsimd", lambda e: e.affine_select(out=usf[:], in_=usf[:], pattern=[[1, 128]], compare_op=ALU.is_ge,
                                                fill=0.0, base=-1, channel_multiplier=-1), [usf], [usf])
    pb.op("vector", lambda e: e.tensor_copy(out=usb[:], in_=usf[:]), [usf], [usb])
    RS = pb.bind.get("rstate")
    iota_i = pb.sb("iota_i", [128, 256], mybir.dt.int32)
    iota = pb.sb("iota", [128, 256]) if RS is None else RS["iota"]
    pb.op("gpsimd", lambda e: e.iota(iota_i[:], pattern=[[1, 256]], base=0, channel_multiplier=0), [], [iota_i])
    pb.op("vector", lambda e: e.tensor_copy(out=iota[:], in_=iota_i[:]), [iota_i], [iota])

    wg_t = [pb.sb("wg%d" % i, [128, 16, 256], BF16) for i in range(2)]
    wu_t = [pb.sb("wu%d" % i, [128, 16, 256], BF16) for i in range(2)]
    wd_t = [pb.sb("wd%d" % i, [128, 2, D], BF16) for i in range(4)]

    def load_gu(e, fp):
        if e >= NEL:
            return
        slot = fp % 2
        src_g = wgd.t[e].rearrange("(k p) f -> p k f", p=128)[:, :, fp * 256:(fp + 1) * 256]
        src_u = wud.t[e].rearrange("(k p) f -> p k f", p=128)[:, :, fp * 256:(fp + 1) * 256]
        pb.dma("gpsimd", wg_t[slot][:], src_g, [wgd], wg_t[slot])
        pb.dma("gpsimd", wu_t[slot][:], src_u, [wud], wu_t[slot])

    def load_d(e):
        if e >= NEL:
            return
        for fp in range(4):
            src = wdd.t[e].rearrange("(c p) d -> p c d", p=128)[:, fp * 2:(fp + 1) * 2, :]
            pb.dma("gpsimd", wd_t[fp][:], src, [wdd], wd_t[fp])

    h2s = pb.sb("h2s", [128, NJ, D], BF16)
    for j in range(NJ):
        if "h2p" in pb.bind:
            hp = pb.bind["h2p"][(j % 8) // 4]
            off = (j % 4) * 128
            pb.dma("sync", h2s[:, j, :], hp.t[j // 8, off:off + 128, :], [hp], h2s)
        else:
            pb.dma("sync", h2s[:, j, :], h2d[j * 128:(j + 1) * 128, :], [h2d], h2s)
    load_gu(0, 0)
    load_gu(0, 1)
    load_d(0)

    work = pb.sb("work", [NEL, S])
    m8 = pb.sb("m8", [NEL, 8])
    pb.dma("sync", work[:], affTd[:], [affTd], work)
    for it in range(CAP // 8):
        pb.op("vector", lambda e: e.max(out=m8[:], in_=work[:]), [work], [m8])
        if it < CAP // 8 - 1:
            pb.op("vector", lambda e: e.match_replace(out=work[:], in_to_replace=m8[:], in_values=work[:], imm_value=-1.0),
                  [work, m8], [work])
    diag8 = pb.sb("diag8", [NEL, NEL])
    pb.op("vector", lambda e: e.tensor_scalar(out=diag8[:], in0=identf[0:NEL, 0:NEL], scalar1=m8[:, 7:8], scalar2=None,
                                               op0=ALU.mult), [identf, m8], [diag8])
    pb.mm(PS[0][:, 0:NEL], ones_f[0:NEL, :], diag8[:], True, True, [ones_f, diag8], [PS[0]])
    thrB = pb.sb("thrB", [128, NEL])
    pb.op("vector", lambda e: e.tensor_copy(out=thrB[:], in_=PS[0][:, 0:NEL]), [PS[0]], [thrB])
    afft = pb.sb("afft", [128, NJ, NEL])
    pb.dma("sync", afft[:], afftd.t.rearrange("(j p) e -> p j e", p=128), [afftd], afft)
    mask = pb.sb("mask", [128, NJ, NEL])
    maskb = pb.sb("maskb", [128, NJ, NEL], BF16)
    gm = pb.sb("gm", [128, NJ, NEL]) if RS is None else RS["gm"]
    for j in range(NJ):
        pb.op("vector", lambda e, j=j: e.tensor_tensor(out=mask[:, j, :], in0=afft[:, j, :], in1=thrB[:], op=ALU.is_ge),
              [afft, thrB], [mask])
    pb.op("vector", lambda e: e.tensor_copy(out=maskb[:], in_=mask[:]), [mask], [maskb])
    pb.op("vector", lambda e: e.tensor_tensor(out=gm[:], in0=mask[:], in1=afft[:], op=ALU.mult), [mask, afft], [gm])
    ppos = PS[1]
    pposv = ppos[:, 0:NJ * NEL].rearrange("p (j e) -> p j e", e=NEL)
    for j in range(NJ):
        pb.mm(pposv[:, j, :], usb[:], maskb[:, j, :], True, j == 0, [usb, maskb], [ppos])
        for j2 in range(j):
            pb.mm(pposv[:, j, :], ones_b[:], maskb[:, j2, :], False, j2 == j - 1, [ones_b, maskb], [ppos])
    pos = pb.sb("pos", [128, NJ, NEL]) if RS is None else RS["pos"]
    pb.op("vector", lambda e: e.tensor_copy(out=pos[:], in_=pposv), [ppos], [pos])

    P01 = pb.sb("P01", [128, NJ, CAP], BF16)
    xeT = [pb.sb("xeT%d" % i, [128, 16, CAP], BF16) for i in range(2)]
    hidT = pb.sb("hidT", [128, 8, CAP], BF16)
    sil = [pb.sb("sil%d" % i, [128, CAP]) for i in range(2)]
    yst = [pb.sb("yst%d" % i, [128, D], BF16) for i in range(2)]
    ev = 0
    for e_ in range(NEL):
        xe = xeT[e_ % 2]
        for j in range(NJ):
            pb.op("vector", lambda e, j=j, e_=e_: e.tensor_scalar(
                out=P01[:, j, :], in0=iota[:], scalar1=pos[:, j, e_:e_ + 1], scalar2=mask[:, j, e_:e_ + 1],
                op0=ALU.is_equal, op1=ALU.mult), [iota, pos, mask], [P01])
        for dc in range(16):
            pg = PS[dc % 4]
            for j in range(NJ):
                pb.mm(pg[:, 0:CAP], h2s[:, j, dc * 128:(dc + 1) * 128], P01[:, j, :], j == 0, j == NJ - 1, [h2s, P01], [pg])
            if dc % 2 == 0:
                pb.act(None, xe[:, dc, :], pg[:, 0:CAP], AF.Copy, [pg], [xe])
            else:
                pb.op("vector", lambda e, xe=xe, pg=pg, dc=dc: e.tensor_copy(out=xe[:, dc, :], in_=pg[:, 0:CAP]), [pg], [xe])
        for fp in range(4):
            wg_ = wg_t[fp % 2]
            wu_ = wu_t[fp % 2]
            for f2 in range(2):
                fc = fp * 2 + f2
                pgu = PS[4 + fc % 2]
                for k in range(16):
                    pb.mm(pgu[:, 0:CAP], wg_[:, k, f2 * 128:(f2 + 1) * 128], xe[:, k, :], k == 0, k == 15, [wg_, xe], [pgu])
                for k in range(16):
                    pb.mm(pgu[:, CAP:2 * CAP], wu_[:, k, f2 * 128:(f2 + 1) * 128], xe[:, k, :], k == 0, k == 15, [wu_, xe], [pgu])
                sl_ = sil[fc % 2]
                pb.act(None, sl_[:], pgu[:, 0:CAP], AF.Silu, [pgu], [sl_])
                pb.op("vector", lambda e, sl_=sl_, pgu=pgu, fc=fc: e.tensor_tensor(
                    out=hidT[:, fc, :], in0=sl_[:], in1=pgu[:, CAP:2 * CAP], op=ALU.mult), [sl_, pgu], [hidT])
            nfp = fp + 2
            if nfp < 4:
                load_gu(e_, nfp)
            else:
                load_gu(e_ + 1, nfp - 4)
        for ct in range(2):
            ys = yst[ct]
            for dj in range(4):
                pd = PS[6 + dj % 2]
                for fc in range(8):
                    wd_ = wd_t[fc // 2]
                    pb.mm(pd[:], hidT[:, fc, ct * 128:(ct + 1) * 128], wd_[:, fc % 2, dj * 512:(dj + 1) * 512],
                          fc == 0, fc == 7, [hidT, wd_], [pd])
                if dj % 2 == 0:
                    pb.act(None, ys[:, dj * 512:(dj + 1) * 512], pd[:], AF.Copy, [pd], [ys])
                else:
                    pb.op("vector", lambda e, ys=ys, pd=pd, dj=dj: e.tensor_copy(out=ys[:, dj * 512:(dj + 1) * 512], in_=pd[:]), [pd], [ys])
            pb.dma("sync", yscr[e_ * 2 + ct], ys[:], [ys], yscr)
        load_d(e_ + 1)

    split = "psend" in pb.bind
    if split:
        bind_ = pb.bind
        pb.end()
        pb.begin(bind_)
        h2s = pb.sb("yes", [128, NJ, D], BF16)
        psend, pown, precv = bind_["psend"], bind_["pown"], bind_["precv"]
        sel = pb.sb("sel", [128, 2])
        osel = pb.sb("osel", [128, 2])
        pb.dma("sync", sel[:], bind_["selv"][:], [bind_["selv"]], sel)
        pb.dma("sync", osel[:], bind_["oselv"][:], [bind_["oselv"]], osel)
        outs2 = [[pb.sb("outt%d_%d" % (h, i), [128, D]) for i in range(2)] for h in range(2)]
        s1s = [pb.sb("s1_%d" % i, [128, D]) for i in range(2)]
        s2s = [pb.sb("s2_%d" % i, [128, D]) for i in range(2)]
    for i in range(NEL * 2):
        pb.dma("sync", h2s[:, i, :], yscr[i], [yscr], h2s)
    Pg = [pb.sb("Pg%d" % i, [128, CAP], BF16) for i in range(2)]
    PgT = [pb.sb("PgT%d" % i, [128, NEL, CAP], BF16) for i in range(2)]
    outt1 = pb.sb("outt", [128, D]) if not split else None
    c = 0
    order = [(jj, h) for jj in range(8) for h in range(2)] if split else [(j, None) for j in range(NJ)]
    for it, (jj, h) in enumerate(order):
        j = jj if h is None else h * 8 + jj
        outt = outt1 if h is None else outs2[h][jj % 2]
        pgt = PgT[it % 2]
        for e_ in range(NEL):
            pg_ = Pg[c % 2]
            ptp = PS[c % 4]
            c += 1
            pb.op("vector", lambda e, pg_=pg_, j=j, e_=e_: e.tensor_scalar(
                out=pg_[:], in0=iota[:], scalar1=pos[:, j, e_:e_ + 1], scalar2=gm[:, j, e_:e_ + 1],
                op0=ALU.is_equal, op1=ALU.mult), [iota, pos, gm], [pg_])
            for ct in range(2):
                pb.mm(ptp[:, ct * 128:(ct + 1) * 128], pg_[:, ct * 128:(ct + 1) * 128], identb[:], True, True, [pg_, identb], [ptp])
            pb.act(None, pgt[:, e_, :], ptp[:, 0:CAP], AF.Copy, [ptp], [pgt])
        for dj in range(4):
            po = PS[4 + dj]
            n = 0
            for e_ in range(NEL):
                for ct in range(2):
                    pb.mm(po[:], pgt[:, e_, ct * 128:(ct + 1) * 128], h2s[:, e_ * 2 + ct, dj * 512:(dj + 1) * 512],
                          n == 0, n == NEL * 2 - 1, [pgt, h2s], [po])
                    n += 1
            if dj % 2 == 0:
                pb.act(None, outt[:, dj * 512:(dj + 1) * 512], po[:], AF.Copy, [po], [outt])
            else:
                pb.op("vector", lambda e, po=po, dj=dj, outt=outt: e.tensor_copy(out=outt[:, dj * 512:(dj + 1) * 512], in_=po[:]), [po], [outt])
        if not split:
            pb.dma("sync", part[j * 128:(j + 1) * 128, :], outt[:], [outt], part)
        elif h == 1:
            A, B = outs2[0][jj % 2], outs2[1][jj % 2]
            s1, s2 = s1s[jj % 2], s2s[jj % 2]
            rows = slice(jj * 128, (jj + 1) * 128)
            pb.op("vector", lambda e, A=A, s1=s1: e.tensor_scalar(out=s1[:], in0=A[:], scalar1=osel[:, 0:1], scalar2=None,
                                                                  op0=ALU.mult), [A, osel], [s1])
            pb.op("vector", lambda e, B=B, s1=s1: e.scalar_tensor_tensor(out=s1[:], in0=B[:], scalar=osel[:, 1:2], in1=s1[:],
                                                                       op0=ALU.mult, op1=ALU.add), [B, osel, s1], [s1])
            pb.op("vector", lambda e, A=A, s2=s2: e.tensor_scalar(out=s2[:], in0=A[:], scalar1=sel[:, 0:1], scalar2=None,
                                                                  op0=ALU.mult), [A, sel], [s2])
            pb.op("vector", lambda e, B=B, s2=s2: e.scalar_tensor_tensor(out=s2[:], in0=B[:], scalar=sel[:, 1:2], in1=s2[:],
                                                                       op0=ALU.mult, op1=ALU.add), [B, sel, s2], [s2])
            pb.dma("sync", psend[rows, :], s1[:], [s1], psend)
            pb.dma("sync", pown[rows, :], s2[:], [s2], pown)
            if jj % 2 == 1:
                pc = jj // 2
                coll_gather(pb, T(psend.t[pc * 256:(pc + 1) * 256, :], psend.b),
                            T(precv[pc].t.rearrange("q t d -> (q t) d"), precv[pc].b))
    return pb.finish() if own else None


def run_L4(nc, h2, aff, w_gate_l, w_up_l, w_down_l):
    maps = []
    for i in range(8):
        b, r = i // 2, i % 2
        es = slice(8 * r, 8 * r + 8)
        maps.append({"h2": h2[b], "affT": np.ascontiguousarray(aff[b][:, es].T), "afft": np.ascontiguousarray(aff[b][:, es]),
                     "wg": w_gate_l[es], "wu": w_up_l[es], "wd": w_down_l[es]})
    res = run(nc, maps)
    return [res[i]["part"] for i in range(8)]


def build_L5(pb=None):
    own = pb is None
    pb = pb or PB()
    x = pb.din("x", [1024, D])
    p0 = pb.din("p0", [1024, D])
    p1 = pb.din("p1", [1024, D])
    g2d = pb.din("g2", [128, D])
    xo = pb.dout("xo", [1024, D])
    g2 = pb.sb("g2s", [128, D])
    pb.dma("sync", g2[:], g2d[:], [g2d], g2)
    xts = [pb.sb("xt%d" % i, [128, D]) for i in range(2)]
    pts = [pb.sb("pt%d" % i, [128, D]) for i in range(2)]
    qts = [pb.sb("qt%d" % i, [128, D]) for i in range(2)]
    for t in range(8):
        xt, pt, qt = xts[t % 2], pts[t % 2], qts[t % 2]
        ts_ = slice(t * 128, (t + 1) * 128)
        pb.dma("sync", xt[:], x[ts_, :], [x], xt)
        pb.dma("sync", pt[:], p0[ts_, :], [p0], pt)
        pb.dma("sync", qt[:], p1[ts_, :], [p1], qt)
        pb.op("vector", lambda e, pt=pt, qt=qt: e.tensor_tensor(out=pt[:], in0=pt[:], in1=qt[:], op=ALU.add), [pt, qt], [pt])
        pb.op("gpsimd", lambda e, pt=pt: e.tensor_tensor(out=pt[:], in0=pt[:], in1=g2[:], op=ALU.mult), [pt, g2], [pt])
        pb.op("vector", lambda e, pt=pt, xt=xt: e.tensor_tensor(out=xt[:], in0=xt[:], in1=pt[:], op=ALU.add), [pt, xt], [xt])
        pb.dma("sync", xo[ts_, :], xt[:], [xt], xo)
    return pb.finish() if own else None


def run_L5(nc, x1, parts, g2):
    maps = []
    for i in range(8):
        b, r = i // 2, i % 2
        sl = slice(r * 1024, (r + 1) * 1024)
        maps.append({"x": np.ascontiguousarray(x1[b, sl]), "p0": np.ascontiguousarray(parts[2 * b][sl]),
                     "p1": np.ascontiguousarray(parts[2 * b + 1][sl]), "g2": rep128(g2[b])})
    res = run(nc, maps)
    out = np.zeros((4, S, D), np.float32)
    for i in range(8):
        b, r = i // 2, i % 2
        out[b, r * 1024:(r + 1) * 1024] = res[i]["xo"]
    return out


def mod_steps(pb, cTd, wd_, brepd, modrep, nchunk, lbase, banks):
    ones_f = pb.sb("ones_f", [128, 128])
    pb.op("gpsimd", lambda e: e.memset(ones_f[:], 1.0), [], [ones_f])
    cs = pb.sb("cs", [128, 16])
    pb.dma("sync", cs[:], cTd[:], [cTd], cs)
    pb.act(None, cs[:], cs[:], AF.Silu, [cs], [cs])
    crep = pb.sb("crep", [128, 16, 128], BF16)
    for k in range(16):
        pb.act(None, crep[:, k, :], ones_f[:], AF.Copy, [ones_f, cs], [crep], scale=cs[:, k:k + 1])
    wv = wd_.t.rearrange("(k p) n -> p k n", p=128)
    wts = [pb.sb("mw%d" % i, [128, 16, 512], BF16) for i in range(2)]
    bss = [pb.sb("mbs%d" % i, [128, 512]) for i in range(2)]
    oss = [pb.sb("mos%d" % i, [128, 512]) for i in range(2)]
    for j in range(nchunk):
        wt, bs, os_, pst = wts[j % 2], bss[j % 2], oss[j % 2], banks[j % 2]
        pb.dma("gpsimd", wt[:], wv[:, :, j * 512:(j + 1) * 512], [wd_], wt)
        pb.dma("sync", bs[:], brepd[:, j * 512:(j + 1) * 512], [brepd], bs)
        for k in range(16):
            pb.mm(pst[:], crep[:, k, :], wt[:, k, :], k == 0, k == 15, [crep, wt], [pst])
        pb.op("vector", lambda e, os_=os_, pst=pst, bs=bs: e.tensor_tensor(out=os_[:], in0=pst[:], in1=bs[:], op=ALU.add),
              [pst, bs], [os_])
        pb.dma("sync", modrep[lbase + j // 4][:, (j % 4) * 512:(j % 4 + 1) * 512], os_[:], [os_], modrep)
        yield j


def emit_mod(pb, l, cTd, wd_, brepd, modrep, nchunk=24, lbase=None):
    if lbase is None:
        lbase = l * 6
    pb.begin({})
    for _ in mod_steps(pb, cTd, wd_, brepd, modrep, nchunk, lbase, pb.banks()[0:2]):
        pass
    pb.end()


def build_fused():
    pb = PB()
    V = lambda parent, ap: T(ap, parent.b)
    x = pb.din("x", [S, D])
    cT = pb.din("cT", [DEPTH * 0 + 128, 16])
    w_ada = pb.din("w_ada", [DEPTH, D, 6 * D])
    brep = pb.din("brep", [DEPTH, 128, 6 * D])
    w_in = pb.din("w_in", [DEPTH, D, INW])
    w_out = pb.din("w_out", [DEPTH, D, D])
    w_router = pb.din("w_router", [DEPTH, D, NE])
    w_gate = pb.din("w_gate", [DEPTH, NE, D, FF])
    w_up = pb.din("w_up", [DEPTH, NE, D, FF])
    w_down = pb.din("w_down", [DEPTH, NE, FF, D])
    cs5 = pb.din("cs5", [S, 640])
    sn5 = pb.din("sn5", [S, 640])
    gain = pb.din("gainr", [DEPTH, 128, 640])
    sinkr = pb.din("sinkrr", [DEPTH * 2, 128, 512])
    bg = pb.din("bgr", [DEPTH * 2, 128, 128])
    mgain = pb.din("mgainr", [DEPTH * 2, 128, 512])
    out = pb.dout("out", [S, D])
    modrep = pb.dscratch("modrep", [DEPTH * 6, 128, D])
    pre = pb.dscratch("pre", [S, INW])
    mixT = pb.dscratch("mixT", [16, 128, S], BF16)
    xa = pb.dscratch("xa", [S, D])
    xb = pb.dscratch("xb", [S, D])
    xc = pb.dscratch("xc", [S, D])
    h2 = pb.dscratch("h2", [S, D], BF16)
    aff = pb.dscratch("aff", [S, NE])
    affT = pb.dscratch("affT", [NE, S])
    parts = [pb.dscratch("part%d" % i, [S, D]) for i in range(2)]
    yscr = pb.dscratch("yscr", [16, 128, D], BF16)
    pb.banks()
    xin = x
    for l in range(DEPTH):
        emit_mod(pb, l, cT, V(w_ada, w_ada[l]), V(brep, brep[l]), modrep)
        M = lambda i: V(modrep, modrep[l * 6 + i])
        x3 = xa if l == 0 else xc
        for r in range(2):
            rs = slice(r * 1024, (r + 1) * 1024)
            b = {"x": V(xin, xin[rs, :]), "sc": M(1), "sh": M(0), "w": V(w_in, w_in[l]), "pre": V(pre, pre[rs, :])}
            if l > 0:
                b.update({"p0": V(parts[0], parts[0][rs, :]), "p1": V(parts[1], parts[1][rs, :]),
                          "g2": V(modrep, modrep[(l - 1) * 6 + 5]), "xo": V(xb, xb[rs, :])})
            pb.begin(b)
            build_L1(l > 0, pb)
            pb.end()
        if l > 0:
            xin = xb
        for r in range(2):
            b = {"aq": V(pre, pre[:, r * 512:(r + 1) * 512]), "ak": V(pre, pre[:, 1024 + r * 128:1024 + (r + 1) * 128]),
                 "cs": cs5, "sn": sn5, "gain": V(gain, gain[l]), "av": V(pre, pre[:, 1280 + r * 128:1280 + (r + 1) * 128]),
                 "sinkr": V(sinkr, sinkr[l * 2 + r]),
                 "mq": V(pre, pre[:, 1536 + r * 256:1536 + (r + 1) * 256]),
                 "mk": V(pre, pre[:, 2048 + r * 256:2048 + (r + 1) * 256]),
                 "mv": V(pre, pre[:, 2560 + r * 512:2560 + (r + 1) * 512]),
                 "mo": V(pre, pre[:, 3584 + r * 512:3584 + (r + 1) * 512]),
                 "gt16": V(pre, pre[:, 4608:4624]), "rsel": r,
                 "bg": V(bg, bg[l * 2 + r]), "mgain": V(mgain, mgain[l * 2 + r]),
                 "attT": V(mixT, mixT[4 * r:4 * r + 4]), "moT": V(mixT, mixT[8 + 4 * r:8 + 4 * r + 4])}
            pb.begin(b)
            build_L2(pb)
            pb.end()
        for r in range(2):
            rs = slice(r * 1024, (r + 1) * 1024)
            b = {"mixT": V(mixT, mixT[:, :, rs]), "x": V(xin, xin[rs, :]), "w": V(w_out, w_out[l]),
                 "g1": M(2), "sc": M(4), "sh": M(3), "wr": V(w_router, w_router[l]),
                 "x1": V(x3, x3[rs, :]), "h2": V(h2, h2[rs, :]), "aff": V(aff, aff[rs, :]), "affT": V(affT, affT[:, rs])}
            pb.begin(b)
            build_L3(pb)
            pb.end()
        for r in range(2):
            es = slice(8 * r, 8 * r + 8)
            b = {"h2": h2, "affT": V(affT, affT[es, :]), "afft": V(aff, aff[:, es]),
                 "wg": V(w_gate, w_gate[l, es]), "wu": V(w_up, w_up[l, es]), "wd": V(w_down, w_down[l, es]),
                 "part": parts[r], "yscr": yscr}
            pb.begin(b)
            build_L4(pb)
            pb.end()
        xin = x3
    for r in range(2):
        rs = slice(r * 1024, (r + 1) * 1024)
        b = {"x": V(xc, xc[rs, :]), "p0": V(parts[0], parts[0][rs, :]), "p1": V(parts[1], parts[1][rs, :]),
             "g2": V(modrep, modrep[(DEPTH - 1) * 6 + 5]), "xo": V(out, out[rs, :])}
        pb.begin(b)
        build_L5(pb)
        pb.end()
    pb.outs = [out]
    nc = pb.finish()
    return nc, pb.S.stats


GROUPS = [[0, 1], [2, 3], [4, 5], [6, 7]]


def coll_gather(pb, src, dst):
    return pb.S.op("gpsimd", lambda e: e.collective_compute("AllGather", ALU.bypass, replica_groups=GROUPS,
                                                            ins=[src.t.opt()], outs=[dst.t.opt()]),
                   reads=[src.b], writes=[dst.b], dma=True, dma_inc=1)


def emit_pack(pb, parto, selv, oselv, psend, pown):
    sel = pb.sb("sel", [128, 2])
    osel = pb.sb("osel", [128, 2])
    pb.dma("sync", sel[:], selv[:], [selv], sel)
    pb.dma("sync", osel[:], oselv[:], [oselv], osel)
    A = [pb.sb("pkA%d" % i, [128, D]) for i in range(2)]
    B = [pb.sb("pkB%d" % i, [128, D]) for i in range(2)]
    o1 = [pb.sb("pko%d" % i, [128, D]) for i in range(2)]
    o2 = [pb.sb("pkp%d" % i, [128, D]) for i in range(2)]
    for jj in range(8):
        a, b_, s1, s2 = A[jj % 2], B[jj % 2], o1[jj % 2], o2[jj % 2]
        rows = slice(jj * 128, (jj + 1) * 128)
        pb.dma("sync", a[:], parto[jj * 128:(jj + 1) * 128, :], [parto], a)
        pb.dma("sync", b_[:], parto[1024 + jj * 128:1024 + (jj + 1) * 128, :], [parto], b_)
        pb.op("vector", lambda e, a=a, s1=s1: e.tensor_scalar(out=s1[:], in0=a[:], scalar1=osel[:, 0:1], scalar2=None,
                                                              op0=ALU.mult), [a, osel], [s1])
        pb.op("vector", lambda e, b_=b_, s1=s1: e.scalar_tensor_tensor(out=s1[:], in0=b_[:], scalar=osel[:, 1:2], in1=s1[:],
                                                                       op0=ALU.mult, op1=ALU.add), [b_, osel, s1], [s1])
        pb.op("vector", lambda e, a=a, s2=s2: e.tensor_scalar(out=s2[:], in0=a[:], scalar1=sel[:, 0:1], scalar2=None,
                                                              op0=ALU.mult), [a, sel], [s2])
        pb.op("vector", lambda e, b_=b_, s2=s2: e.scalar_tensor_tensor(out=s2[:], in0=b_[:], scalar=sel[:, 1:2], in1=s2[:],
                                                                       op0=ALU.mult, op1=ALU.add), [b_, sel, s2], [s2])
        pb.dma("sync", psend[rows, :], s1[:], [s1], psend)
        pb.dma("sync", pown[rows, :], s2[:], [s2], pown)


def emit_recv_tiles(pb, osel, pown, precv, dst_tile, tmp_tiles, rows):
    pc, off = rows.start // 256, rows.start % 256
    pb.dma("sync", dst_tile[:], pown[rows, :], [pown], dst_tile)
    for q in range(2):
        tt = tmp_tiles[q]
        pb.dma("sync", tt[:], precv[pc].t[q, off:off + 128, :], [precv[pc]], tt)
        pb.op("vector", lambda e, tt=tt, q=q: e.scalar_tensor_tensor(
            out=dst_tile[:], in0=tt[:], scalar=osel[:, q:q + 1], in1=dst_tile[:], op0=ALU.mult, op1=ALU.add),
            [tt, osel, dst_tile], [dst_tile])


def emit_blend_tiles(pb, sel, partg_p, dst_tile, tmp_tiles, rows):
    first = True
    i = 0
    for q in range(2):
        for h in range(2):
            tt = tmp_tiles[i % len(tmp_tiles)]
            i += 1
            grow = h * 1024 + rows.start
            pc, off = grow // 256, grow % 256
            pb.dma("sync", tt[:], partg_p[pc].t[q, off:off + 128, :], [partg_p[pc]], tt)
            if first:
                pb.op("vector", lambda e, tt=tt, h=h: e.tensor_scalar(out=dst_tile[:], in0=tt[:], scalar1=sel[:, h:h + 1],
                                                                      scalar2=None, op0=ALU.mult), [tt, sel], [dst_tile])
                first = False
            else:
                pb.op("vector", lambda e, tt=tt, h=h: e.scalar_tensor_tensor(
                    out=dst_tile[:], in0=tt[:], scalar=sel[:, h:h + 1], in1=dst_tile[:], op0=ALU.mult, op1=ALU.add),
                    [tt, sel, dst_tile], [dst_tile])


def emit_h(pb, xsrc, scd, shd, hTs, prol=None):
    NT = 8
    PS = pb.banks()
    ident = make_ident(pb, BF16)
    onepsc = pb.sb("onepsc", [128, D])
    shs = pb.sb("shs", [128, D])
    pb.dma("sync", onepsc[:], scd[:], [scd], onepsc)
    pb.dma("sync", shs[:], shd[:], [shd], shs)
    pb.op("vector", lambda e: e.tensor_scalar(out=onepsc[:], in0=onepsc[:], scalar1=1.0, scalar2=None, op0=ALU.add),
          [onepsc], [onepsc])
    if prol is not None:
        partg, selv, g2d, xo = prol
        sel = pb.sb("sel", [128, 2])
        pb.dma("sync", sel[:], selv[:], [selv], sel)
        g2 = pb.sb("g2s", [128, D])
        pb.dma("sync", g2[:], g2d[:], [g2d], g2)
        tmps = [pb.sb("btmp%d" % i, [128, D]) for i in range(2)]
        pacc = pb.sb("pacc", [128, D])
    hTt = pb.sb("hTt", [128, 16, 1024], BF16)
    xts = [pb.sb("xt%d" % i, [128, D]) for i in range(2)]
    junk = pb.sb("junk", [128, D])
    hb = pb.sb("hb", [128, D], BF16)
    ssq = pb.sb("ssq", [128, 1])
    for t in range(NT):
        xt = xts[t % 2]
        rows = slice(t * 128, (t + 1) * 128)
        pb.dma("sync", xt[:], xsrc[rows, :], [xsrc], xt)
        if prol is not None:
            emit_recv_tiles(pb, sel, partg[0], partg[1], pacc, tmps, rows)
            pb.op("gpsimd", lambda e: e.tensor_tensor(out=pacc[:], in0=pacc[:], in1=g2[:], op=ALU.mult), [pacc, g2], [pacc])
            pb.op("vector", lambda e, xt=xt: e.tensor_tensor(out=xt[:], in0=xt[:], in1=pacc[:], op=ALU.add), [pacc, xt], [xt])
            pb.dma("sync", xo[rows, :], xt[:], [xt], xo)
        if hTs is None:
            continue
        emit_norm_mod(pb, xt, onepsc, shs, junk, ssq, hb)
        for g in range(4):
            pt_ = PS[4 + g % 2]
            for q in range(4):
                k = g * 4 + q
                pb.mm(pt_[:, q * 128:(q + 1) * 128], hb[:, k * 128:(k + 1) * 128], ident[:], True, True, [hb, ident], [pt_])
            pb.act(None, hTt[:, g * 4:(g + 1) * 4, t * 128:(t + 1) * 128],
                   pt_[:].rearrange("p (q n) -> p q n", q=4), AF.Copy, [pt_], [hTt])
    if hTs is not None:
        pb.dma("sync", hTs.t.rearrange("k p t -> p k t"), hTt[:], [hTt], hTs)


PREW = 2312


def emit_inproj(pb, hTg, w, pre):
    for _ in inproj_steps(pb, hTg, w, pre):
        pass


def inproj_steps(pb, hTg, w, pre):
    PS = pb.banks()
    hT = pb.sb("hT", [128, 16, S], BF16)
    for q in range(2):
        for pc in range(2):
            pb.dma("sync", hT[:, pc * 8:(pc + 1) * 8, q * 1024:(q + 1) * 1024],
                   hTg[pc].t[q].rearrange("k p t -> p k t"), [hTg[pc]], hT)
    wv = w.t.rearrange("(k p) n -> p k n", p=128)
    wts = [pb.sb("w%d" % i, [128, 16, 512], BF16) for i in range(2)]
    stg = [pb.sb("stg%d" % i, [128, 512]) for i in range(4)]
    cnt = 0
    for j in range(5):
        c0 = j * 512
        cw = min(512, PREW - c0)
        wt = wts[j % 2]
        pb.dma("gpsimd", wt[:, :, :cw], wv[:, :, c0:c0 + cw], [w], wt)
        for t in range(16):
            pst = PS[cnt % 4]
            st = stg[cnt % 4]
            for k in range(16):
                pb.mm(pst[:, :cw], hT[:, k, t * 128:(t + 1) * 128], wt[:, k, :cw], k == 0, k == 15, [hT, wt], [pst])
            if cnt % 2 == 0:
                pb.op("vector", lambda e, st=st, pst=pst, cw=cw: e.tensor_copy(out=st[:, :cw], in_=pst[:, :cw]), [pst], [st])
            else:
                pb.act(None, st[:, :cw], pst[:, :cw], AF.Copy, [pst], [st])
            pb.dma("sync", pre[t * 128:(t + 1) * 128, c0:c0 + cw], st[:, :cw], [st], pre)
            cnt += 1
        yield j


def emit_affblend(pb, affg, affTg, selv, afft_m, affT_m):
    sel = pb.sb("sel", [128, 2])
    pb.dma("sync", sel[:], selv[:], [selv], sel)
    a = pb.sb("affa", [128, 16, 16])
    pb.dma("sync", a[:], affg.t.rearrange("q (j p) e -> p (q j) e", p=128), [affg], a)
    am = pb.sb("affm", [128, 16, 8])
    pb.op("vector", lambda e: e.tensor_scalar(out=am[:], in0=a[:, :, 0:8], scalar1=sel[:, 0:1], scalar2=None, op0=ALU.mult),
          [a, sel], [am])
    pb.op("vector", lambda e: e.scalar_tensor_tensor(out=am[:], in0=a[:, :, 8:16], scalar=sel[:, 1:2], in1=am[:],
                                                      op0=ALU.mult, op1=ALU.add), [a, sel, am], [am])
    pb.dma("sync", afft_m.t.rearrange("(j p) e -> p j e", p=128), am[:], [am], afft_m)
    tl = pb.sb("affTl", [8, 2, 1024])
    th = pb.sb("affTh", [8, 2, 1024])
    pb.dma("sync", tl[:], affTg.t[:, 0:8, :].rearrange("q e t -> e q t"), [affTg], tl)
    pb.dma("sync", th[:], affTg.t[:, 8:16, :].rearrange("q e t -> e q t"), [affTg], th)
    tm = pb.sb("affTm", [8, 2, 1024])
    pb.op("vector", lambda e: e.tensor_scalar(out=tm[:], in0=tl[:], scalar1=sel[0:8, 0:1], scalar2=None, op0=ALU.mult),
          [tl, sel], [tm])
    pb.op("vector", lambda e: e.scalar_tensor_tensor(out=tm[:], in0=th[:], scalar=sel[0:8, 1:2], in1=tm[:],
                                                      op0=ALU.mult, op1=ALU.add), [th, sel, tm], [tm])
    pb.dma("sync", affT_m.t.rearrange("e (q t) -> e q t", q=2), tm[:], [tm], affT_m)


def build_fused8():
    pb = PB()
    V = lambda parent, ap: T(ap, parent.b)
    x = pb.din("x", [1024, D])
    cT = pb.din("cT", [128, 16])
    selv = pb.din("selv", [128, 2])
    w_ada = pb.din("w_ada", [DEPTH, D, 3 * D])
    brep = pb.din("brep", [DEPTH, 128, 3 * D])
    oselv = pb.din("oselv", [128, 2])
    w_in = pb.din("w_in", [DEPTH, D, PREW])
    w_out = pb.din("w_out", [DEPTH, D, D])
    w_router = pb.din("w_router", [DEPTH, D, NE])
    w_gate = pb.din("w_gate", [DEPTH, 8, D, FF])
    w_up = pb.din("w_up", [DEPTH, 8, D, FF])
    w_down = pb.din("w_down", [DEPTH, 8, FF, D])
    cs5 = pb.din("cs5", [S, 640])
    sn5 = pb.din("sn5", [S, 640])
    gain = pb.din("gainr", [DEPTH, 128, 640])
    sinkr = pb.din("sinkrr", [DEPTH, 128, 512])
    bg = pb.din("bgr", [DEPTH, 128, 128])
    mgain = pb.din("mgainr", [DEPTH, 128, 512])
    out = pb.dout("out", [1024, D])
    modh = pb.dscratch("modh", [3, 128, D])
    modg = [[pb.dscratch("modg%d_%d" % (l, i), [2, 128, D]) for i in range(3)] for l in range(DEPTH)]
    psend = pb.dscratch("psend", [1024, D])
    pown = pb.dscratch("pown", [1024, D])
    precv = [pb.dscratch("precv%d" % i, [2, 256, D]) for i in range(4)]
    hTs = pb.dscratch("hTs", [16, 128, 1024], BF16)
    hTg = [pb.dscratch("hTg%d" % i, [2, 8, 128, 1024], BF16) for i in range(2)]
    pre = pb.dscratch("pre", [S, PREW])
    mixo = pb.dscratch("mixo", [8, 128, S], BF16)
    mixg = [pb.dscratch("mixg%d" % i, [2, 4, 128, S], BF16) for i in range(2)]
    xa = pb.dscratch("xa", [1024, D])
    xb = pb.dscratch("xb", [1024, D])
    xc = pb.dscratch("xc", [1024, D])
    h2o = pb.dscratch("h2o", [1024, D], BF16)
    h2g = [pb.dscratch("h2g%d" % i, [2, 512, D], BF16) for i in range(2)]
    affo = pb.dscratch("affo", [1024, NE])
    affg = pb.dscratch("affg", [2, 1024, NE])
    affTo = pb.dscratch("affTo", [NE, 1024])
    affTg = pb.dscratch("affTg", [2, NE, 1024])
    afft_m = pb.dscratch("afftm", [S, 8])
    affT_m = pb.dscratch("affTm", [8, S])
    parto = pb.dscratch("parto", [S, D])
    partg = [pb.dscratch("partg%d" % i, [2, 256, D]) for i in range(8)]
    yscr = pb.dscratch("yscr", [16, 128, D], BF16)
    pb.banks()
    rstate = {"iota": pb.sb("r_iota", [128, 256]), "identb": pb.sb("r_identb", [128, 128], BF16),
              "gm": pb.sb("r_gm", [128, 16, 8]), "pos": pb.sb("r_pos", [128, 16, 8])}
    xin = x
    for l in range(DEPTH):
        if l == 0:
            emit_mod(pb, l, cT, V(w_ada, w_ada[l]), V(brep, brep[l]), modh, nchunk=12, lbase=0)
            pb.begin({})
            for i in range(3):
                coll_gather(pb, V(modh, modh[i]), V(modg[l][i], modg[l][i].t.rearrange("q p d -> (q p) d")))
            pb.end()
        Ml = lambda ll, i: V(modg[ll][i % 3], modg[ll][i % 3].t[i // 3])
        M = lambda i: Ml(l, i)
        x3 = xa if l == 0 else xc
        pb.begin({})
        prol = None if l == 0 else ((pown, precv), oselv, Ml(l - 1, 5), xb)
        emit_h(pb, xin, M(1), M(0), hTs, prol)
        pb.end()
        if l > 0:
            xin = xb
        pb.begin({})
        for pc in range(2):
            coll_gather(pb, V(hTs, hTs.t.rearrange("k p t -> (k p) t")[pc * 1024:(pc + 1) * 1024, :]),
                        V(hTg[pc], hTg[pc].t.rearrange("q k p t -> (q k p) t")))
        if l + 1 < DEPTH:
            g1 = inproj_steps(pb, hTg, V(w_in, w_in[l]), pre)
            g2 = mod_steps(pb, cT, V(w_ada, w_ada[l + 1]), V(brep, brep[l + 1]), modh, 12, 0, pb.banks()[6:8])
            for n2 in (3, 3, 2, 2, 2):
                next(g1)
                for _ in range(n2):
                    next(g2)
            for _ in g1:
                pass
            for _ in g2:
                pass
        else:
            emit_inproj(pb, hTg, V(w_in, w_in[l]), pre)
        pb.end()
        if l + 1 < DEPTH:
            pb.begin({})
            for i in range(3):
                coll_gather(pb, V(modh, modh[i]), V(modg[l + 1][i], modg[l + 1][i].t.rearrange("q p d -> (q p) d")))
            pb.end()
        b = {"aq": V(pre, pre[:, 0:512]), "ak": V(pre, pre[:, 512:640]), "cs": cs5, "sn": sn5, "gain": V(gain, gain[l]),
             "av": V(pre, pre[:, 640:768]), "sinkr": V(sinkr, sinkr[l]),
             "mq": V(pre, pre[:, 768:1024]), "mk": V(pre, pre[:, 1024:1280]), "mv": V(pre, pre[:, 1280:1792]),
             "mo": V(pre, pre[:, 1792:2304]), "gt": V(pre, pre[:, 2304:2312]),
             "bg": V(bg, bg[l]), "mgain": V(mgain, mgain[l]),
             "attT": V(mixo, mixo[0:4]), "moT": V(mixo, mixo[4:8])}
        pb.begin(b)
        build_L2(pb)
        pb.end()
        b = {"mixg": mixg, "selv": selv, "x": xin, "w": V(w_out, w_out[l]),
             "g1": M(2), "sc": M(4), "sh": M(3), "wr": V(w_router, w_router[l]),
             "x1": x3, "h2": h2o, "aff": affo, "affT": affTo}
        pb.begin(b)
        for pc in range(2):
            coll_gather(pb, V(mixo, mixo.t.rearrange("c p t -> (c p) t")[pc * 512:(pc + 1) * 512, :]),
                        V(mixg[pc], mixg[pc].t.rearrange("q c p t -> (q c p) t")))
        build_L3(pb)
        pb.end()
        pb.begin({})
        coll_gather(pb, affo, V(affg, affg.t.rearrange("q t e -> (q t) e")))
        coll_gather(pb, affTo, V(affTg, affTg.t.rearrange("q e t -> (q e) t")))
        pb.end()
        pb.begin({})
        emit_affblend(pb, affg, affTg, selv, afft_m, affT_m)
        pb.end()
        b = {"h2p": h2g, "affT": affT_m, "afft": afft_m,
             "wg": V(w_gate, w_gate[l]), "wu": V(w_up, w_up[l]), "wd": V(w_down, w_down[l]),
             "part": parto, "yscr": yscr, "rstate": rstate, "psend": psend, "pown": pown, "precv": precv,
             "selv": selv, "oselv": oselv}
        pb.begin(b)
        for pc in range(2):
            coll_gather(pb, V(h2o, h2o[pc * 512:(pc + 1) * 512, :]), V(h2g[pc], h2g[pc].t.rearrange("q t d -> (q t) d")))
        build_L4(pb)
        pb.end()
        xin = x3
    pb.begin({})
    emit_h(pb, xc, Ml(0, 0), Ml(0, 0), None, ((pown, precv), oselv, Ml(DEPTH - 1, 5), out))
    pb.end()
    pb.outs = [out]
    nc = pb.finish()
    return nc, pb.S.stats


def in_cols(r):
    ar = np.arange
    gates = (4608 + (ar(4)[:, None] * 4 + (2 * r + ar(2))[None, :])).reshape(-1)
    return np.concatenate([ar(r * 512, (r + 1) * 512), 1024 + ar(r * 128, (r + 1) * 128), 1280 + ar(r * 128, (r + 1) * 128),
                           1536 + ar(r * 256, (r + 1) * 256), 2048 + ar(r * 256, (r + 1) * 256),
                           2560 + ar(r * 512, (r + 1) * 512), 3584 + ar(r * 512, (r + 1) * 512), gates])


_NC = {}


def kernel(x, c, w_ada, b_ada, w_in, b_gates, q_gain, k_gain, sink, m_gain, w_out, w_router, w_gate, w_up, w_down):
    f = lambda a: np.ascontiguousarray(np.asarray(a, dtype=np.float32))
    x, c, w_ada, b_ada, w_in, b_gates = f(x), f(c), f(w_ada), f(b_ada), f(w_in), f(b_gates)
    q_gain, k_gain, sink, m_gain, w_out, w_router = f(q_gain), f(k_gain), f(sink), f(m_gain), f(w_out), f(w_router)
    w_gate, w_up, w_down = f(w_gate), f(w_up), f(w_down)
    if "nc" not in _NC:
        _NC["nc"] = build_fused8()[0]
    nc = _NC["nc"]
    cs5, sn5 = rope_tables_np()
    brep = np.stack([rep128(b_ada[l]) for l in range(DEPTH)])
    gainr = np.stack([rep128(np.concatenate([np.tile(q_gain[l], 4), k_gain[l]])) for l in range(DEPTH)])
    per_r = []
    for r in range(2):
        es = slice(8 * r, 8 * r + 8)
        selv = np.zeros((128, 2), np.float32)
        selv[:, r] = 1.0
        per_r.append({
            "selv": selv, "oselv": np.ascontiguousarray(1.0 - selv),
            "w_ada": np.ascontiguousarray(w_ada[:, :, r * 3 * D:(r + 1) * 3 * D]),
            "brep": np.stack([rep128(b_ada[l][r * 3 * D:(r + 1) * 3 * D]) for l in range(DEPTH)]),
            "w_in": np.ascontiguousarray(w_in[:, :, in_cols(r)]),
            "w_gate": np.ascontiguousarray(w_gate[:, es]), "w_up": np.ascontiguousarray(w_up[:, es]),
            "w_down": np.ascontiguousarray(w_down[:, es]),
            "sinkrr": np.stack([rep128(np.repeat(sink[l][4 * r:4 * r + 4], 128)) for l in range(DEPTH)]),
            "bgr": np.stack([rep128(np.tile(b_gates[l][:, 2 * r:2 * r + 2].reshape(8), 16)) for l in range(DEPTH)]),
            "mgainr": np.stack([rep128(m_gain[l][r * 512:(r + 1) * 512]) for l in range(DEPTH)]),
        })
    maps = []
    for i in range(8):
        b, r = i // 2, i % 2
        m = {"x": np.ascontiguousarray(x[b, r * 1024:(r + 1) * 1024]), "cT": np.ascontiguousarray(c[b].reshape(16, 128).T),
             "w_out": w_out, "w_router": w_router, "cs5": cs5, "sn5": sn5, "gainr": gainr}
        m.update(per_r[r])
        maps.append(m)
    res = run(nc, maps)
    out = np.zeros((NB, S, D), np.float32)
    for i in range(8):
        b, r = i // 2, i % 2
        out[b, r * 1024:(r + 1) * 1024] = res[i]["out"]
    return out
```

```python
import math
from contextlib import ExitStack
import numpy as np
import ml_dtypes
import concourse.bass as bass
import concourse.mybir as mybir
from concourse.bass_utils import run_bass_kernel_spmd

F32 = mybir.dt.float32
BF16 = mybir.dt.bfloat16
AF = mybir.ActivationFunctionType
ALU = mybir.AluOpType
AX = mybir.AxisListType
NPBF = ml_dtypes.bfloat16

D = 2048
S = 2048
NB = 4
DEPTH = 2
INW = 4624
NE = 16
FF = 1024
CAP = 256
EPS = 1e-6
ENGINES = ("tensor", "vector", "scalar", "gpsimd", "sync")


class Buf:
    __slots__ = ("name", "last_w", "readers", "dma_sem", "dma_cnt", "nowaw", "persistent")

    def __init__(self, name):
        self.name = name
        self.nowaw = False
        self.persistent = False
        self.last_w = None
        self.readers = []
        self.dma_sem = None
        self.dma_cnt = 0


class Op:
    __slots__ = ("eng", "fn", "is_dma", "deps", "signal", "semval", "dma_buf", "dma_val", "idx", "dma_inc", "phase", "dsem")


class Sched:
    def __init__(self, nc):
        self.nc = nc
        self.ops = []
        self.bufs = []
        self.phase = 0
        self.fence_start = 0

    def fence(self):
        deps = set()
        last = {}
        for o in self.ops[self.fence_start:]:
            if o.fn is None:
                continue
            if o.is_dma:
                deps.add(o.idx)
            else:
                last[o.eng] = o.idx
        deps.update(last.values())
        for e in ENGINES:
            o = Op()
            o.eng, o.fn, o.is_dma = e, None, False
            o.dma_inc = 16
            o.idx = len(self.ops)
            o.signal = False
            o.semval = None
            o.dma_buf = None
            o.dma_val = None
            o.dsem = None
            o.phase = self.phase
            o.deps = set(deps)
            self.ops.append(o)
        self.phase += 1
        self.fence_start = len(self.ops)

    def buf(self, name):
        b = Buf(name)
        self.bufs.append(b)
        return b

    def op(self, eng, fn, reads=(), writes=(), dma=False, dma_inc=16):
        o = Op()
        o.eng, o.fn, o.is_dma = eng, fn, dma
        o.dma_inc = dma_inc
        o.phase = self.phase
        o.dsem = None
        o.idx = len(self.ops)
        o.signal = False
        o.semval = None
        o.dma_buf = None
        o.dma_val = None
        deps = set()
        for b in reads:
            if b.last_w is not None:
                deps.add(b.last_w)
        for b in writes:
            if b.nowaw:
                continue
            if b.last_w is not None:
                deps.add(b.last_w)
            deps.update(b.readers)
        o.deps = deps
        for b in reads:
            b.readers.append(o.idx)
        for b in writes:
            b.last_w = o.idx
            b.readers = []
        if dma:
            assert len(writes) == 1
            o.dma_buf = writes[0]
        self.ops.append(o)
        return o

    def emit(self, final_wait_bufs=()):
        nc = self.nc
        ops = self.ops
        for o in ops:
            for d in o.deps:
                p = ops[d]
                if p.is_dma:
                    continue
                if p.eng == o.eng and p.eng == "tensor":
                    continue
                p.signal = True
        per_eng = {e: [] for e in ENGINES}
        for o in ops:
            per_eng[o.eng].append(o)
        cnt = {e: 0 for e in ENGINES}
        pers = {}
        pool_cnt = []
        phase_slots = {}
        cur_phase = -1
        sem_of = {}
        pers_cnt = []
        for o in ops:
            if o.fn is None:
                continue
            if o.is_dma:
                b = o.dma_buf
                if b.persistent:
                    if id(b) not in pers:
                        pers[id(b)] = len(pers_cnt)
                        pers_cnt.append(0)
                    sl = pers[id(b)]
                    pers_cnt[sl] += o.dma_inc
                    o.dma_val = pers_cnt[sl]
                    o.dsem = ("P", sl)
                else:
                    if o.phase != cur_phase:
                        cur_phase = o.phase
                        phase_slots = {}
                    if id(b) not in phase_slots:
                        phase_slots[id(b)] = len(phase_slots)
                        if len(pool_cnt) < len(phase_slots):
                            pool_cnt.append(0)
                    sl = phase_slots[id(b)]
                    pool_cnt[sl] += o.dma_inc
                    o.dma_val = pool_cnt[sl]
                    o.dsem = ("Q", sl)
                b.dma_cnt = o.dma_val
                b.dma_sem = o.dsem
            elif o.signal:
                cnt[o.eng] += 1
                o.semval = cnt[o.eng]
        self.stats = dict(cnt=cnt, n_ops=len(ops), n_pers=len(pers_cnt), n_pool=len(pool_cnt))
        with ExitStack() as es:
            esem = {e: es.enter_context(nc.semaphore("s_" + e)) for e in ENGINES if e != "sync"}
            psems = {("P", i): es.enter_context(nc.semaphore("dp%d" % i)) for i in range(len(pers_cnt))}
            psems.update({("Q", i): es.enter_context(nc.semaphore("dq%d" % i)) for i in range(len(pool_cnt))})
            block = es.enter_context(nc.Block())

            def make(engname):
                def body(eng):
                    seen = {}
                    for o in per_eng[engname]:
                        waits = {}
                        for d in o.deps:
                            p = ops[d]
                            if p.is_dma:
                                key = p.dsem
                                sem = psems[p.dsem]
                                val = p.dma_val
                            else:
                                if p.eng == engname and engname == "tensor":
                                    continue
                                key = ("e", p.eng)
                                sem = esem[p.eng]
                                val = p.semval
                            if seen.get(key, 0) >= val:
                                continue
                            if key not in waits or waits[key][1] < val:
                                waits[key] = (sem, val)
                        for key, (sem, val) in waits.items():
                            eng.wait_ge(sem, val)
                            seen[key] = val
                        if o.fn is None:
                            continue
                        ins = o.fn(eng)
                        if o.is_dma:
                            ins.then_inc(psems[o.dsem], o.dma_inc)
                        elif o.signal:
                            ins.then_inc(esem[engname], 1)
                    if engname == "sync":
                        for b in final_wait_bufs:
                            eng.wait_ge(psems[b.dma_sem], b.dma_cnt)
                return body

            for e in ENGINES:
                if per_eng[e] or e == "sync":
                    getattr(block, e)(make(e))


class T:
    __slots__ = ("t", "b")

    def __init__(self, t, b):
        self.t = t
        self.b = b

    def __getitem__(self, k):
        return self.t[k]


class PB:
    def __init__(self):
        self.nc = bass.Bass("TRN2", target_bir_lowering=False)
        self.S = Sched(self.nc)
        self.es = ExitStack()
        self.outs = []
        self._n = 0
        self.bind = {}
        self.fused = False
        self.pes = None
        self._banks = None

    def banks(self):
        if self._banks is None:
            self._banks = []
            for i in range(8):
                t = self.es.enter_context(self.nc.psum_tensor("bank%d" % i, [128, 512], F32))
                self._banks.append(T(t, self.S.buf("bank%d" % i)))
        return self._banks

    def begin(self, bind):
        self.bind = bind
        self.pes = ExitStack()

    def end(self):
        self.S.fence()
        self.pes.close()
        self.pes = None
        self.bind = {}

    def _name(self, n):
        self._n += 1
        return "%s_%d" % (n, self._n)

    def sb(self, name, shape, dt=F32):
        st = self.pes if self.pes is not None else self.es
        t = st.enter_context(self.nc.sbuf_tensor(self._name(name), list(shape), dt))
        return T(t, self.S.buf(name))

    def ps(self, name, shape, dt=F32):
        t = self.es.enter_context(self.nc.psum_tensor(self._name(name), list(shape), dt))
        return T(t, self.S.buf(name))

    def din(self, name, shape, dt=F32):
        if name in self.bind:
            return self.bind[name]
        t = self.nc.dram_tensor(name, list(shape), dt, kind="ExternalInput").ap()
        return T(t, self.S.buf(name))

    def dout(self, name, shape, dt=F32):
        if name in self.bind:
            return self.bind[name]
        t = self.nc.dram_tensor(name, list(shape), dt, kind="ExternalOutput").ap()
        r = T(t, self.S.buf(name))
        r.b.nowaw = True
        r.b.persistent = True
        self.outs.append(r)
        return r

    def dscratch(self, name, shape, dt=F32):
        if name in self.bind:
            return self.bind[name]
        t = self.nc.dram_tensor(self._name(name), list(shape), dt, kind="Internal").ap()
        r = T(t, self.S.buf(name))
        r.b.nowaw = True
        r.b.persistent = True
        return r

    def dma(self, eng, out, in_, r, w, **kw):
        return self.S.op(eng, lambda e: e.dma_start(out=out, in_=in_, **kw), reads=[x.b for x in r],
                         writes=[w.b], dma=True)

    def op(self, eng, fn, r, w):
        return self.S.op(eng, fn, reads=[x.b for x in r], writes=[x.b for x in w])

    def mm(self, out, lhsT, rhs, start, stop, r, w):
        return self.op("tensor", lambda e: e.matmul(out, lhsT=lhsT, rhs=rhs, start=start, stop=stop), r, w)

    def act(self, eng_unused, out, in_, func, r, w, **kw):
        return self.op("scalar", lambda e: e.activation(out=out, in_=in_, func=func, **kw), r, w)

    def finish(self):
        self.S.emit(final_wait_bufs=[o.b for o in self.outs])
        self.es.close()
        return self.nc


def run(pb_nc, in_maps):
    res = run_bass_kernel_spmd(pb_nc, in_maps, core_ids=list(range(len(in_maps))))
    return res.results


def rep128(v):
    v = np.asarray(v, dtype=np.float32).reshape(1, -1)
    return np.ascontiguousarray(np.broadcast_to(v, (128, v.shape[1])))


def make_ident(pb, dt):
    idf = pb.sb("idf", [128, 128], F32)
    pb.op("gpsimd", lambda e: e.memset(idf[:], 1.0), [], [idf])
    pb.op("gpsimd", lambda e: e.affine_select(out=idf[:], in_=idf[:], pattern=[[-1, 128]],
                                                compare_op=ALU.is_equal, fill=0.0, base=0,
                                                channel_multiplier=1), [idf], [idf])
    if dt == F32:
        return idf
    idb = pb.sb("idb", [128, 128], dt)
    pb.op("vector", lambda e: e.tensor_copy(out=idb[:], in_=idf[:]), [idf], [idb])
    return idb


def tri_mask(pb, name, op, dt):
    mf = pb.sb(name + "f", [128, 128], F32)
    pb.op("gpsimd", lambda e: e.memset(mf[:], 1.0), [], [mf])
    if op == ALU.is_le:
        pat, cm = [[1, 128]], -1
    else:
        pat, cm = [[-1, 128]], 1
    pb.op("gpsimd", lambda e: e.affine_select(out=mf[:], in_=mf[:], pattern=pat,
                                                compare_op=ALU.is_ge, fill=0.0, base=0,
                                                channel_multiplier=cm), [mf], [mf])
    if dt == F32:
        return mf
    mb = pb.sb(name, [128, 128], dt)
    pb.op("vector", lambda e: e.tensor_copy(out=mb[:], in_=mf[:]), [mf], [mb])
    return mb


def build_L0():
    pb = PB()
    NCOL = 3072
    cT = pb.din("cT", [128, 16, 4])
    w = pb.din("w", [D, NCOL])
    brep = pb.din("brep", [4, NCOL])
    out = pb.dout("out", [4, NCOL])
    cs = pb.sb("cs", [128, 16, 4])
    cb = pb.sb("cb", [128, 16, 4], BF16)
    bs = pb.sb("bs", [4, NCOL])
    os_ = pb.sb("os", [4, NCOL])
    pb.dma("sync", cs[:], cT[:], [cT], cs)
    pb.dma("sync", bs[:], brep[:], [brep], bs)
    pb.act(None, cb[:], cs[:], AF.Silu, [cs], [cb])
    wv = w.t.rearrange("(k p) n -> p k n", p=128)
    wts = [pb.sb("w%d" % i, [128, 16, 512], BF16) for i in range(2)]
    pss = [pb.ps("ps%d" % i, [4, 512]) for i in range(2)]
    for j in range(6):
        wt = wts[j % 2]
        pst = pss[j % 2]
        pb.dma("gpsimd", wt[:], wv[:, :, j * 512:(j + 1) * 512], [w], wt)
        for k in range(16):
            pb.mm(pst[:], cb[:, k, :], wt[:, k, :], k == 0, k == 15, [cb, wt], [pst])
        pb.op("vector", lambda e, j=j, pst=pst: e.tensor_tensor(out=os_[:, j * 512:(j + 1) * 512], in0=pst[:],
                                                               in1=bs[:, j * 512:(j + 1) * 512], op=ALU.add),
              [pst, bs], [os_])
    pb.dma("sync", out[:], os_[:], [os_], out)
    return pb.finish()


def run_L0(c, w_ada, b_ada):
    nc = build_L0()
    cT = np.ascontiguousarray(c.reshape(4, 16, 128).transpose(2, 1, 0))
    maps = []
    for i in range(8):
        l, q = i // 4, i % 4
        maps.append({"cT": cT, "w": np.ascontiguousarray(w_ada[l][:, q * 3072:(q + 1) * 3072]),
                     "brep": np.ascontiguousarray(np.broadcast_to(b_ada[l][q * 3072:(q + 1) * 3072], (4, 3072)))})
    res = run(nc, maps)
    mod = np.zeros((DEPTH, 4, 6 * D), np.float32)
    for i in range(8):
        l, q = i // 4, i % 4
        mod[l][:, q * 3072:(q + 1) * 3072] = res[i]["out"]
    return mod


def emit_norm_mod(pb, xt, onepsc, sh, junk, ssq, hout):
    pb.act(None, junk[:], xt[:], AF.Square, [xt], [junk, ssq], accum_out=ssq[:])
    pb.act(None, ssq[:], ssq[:], AF.Sqrt, [ssq], [ssq], bias=EPS, scale=1.0 / D)
    pb.op("vector", lambda e: e.reciprocal(out=ssq[:], in_=ssq[:]), [ssq], [ssq])
    pb.op("vector", lambda e: e.scalar_tensor_tensor(out=junk[:], in0=xt[:], scalar=ssq[:, 0:1], in1=onepsc[:],
                                                       op0=ALU.mult, op1=ALU.mult), [xt, ssq, onepsc], [junk])
    pb.op("gpsimd", lambda e: e.tensor_tensor(out=hout[:], in0=junk[:], in1=sh[:], op=ALU.add), [junk, sh], [hout])


def build_L1(prologue, pb=None):
    own = pb is None
    pb = pb or PB()
    NT = 8
    x = pb.din("x", [1024, D])
    sc = pb.din("sc", [128, D])
    shd = pb.din("sh", [128, D])
    w = pb.din("w", [D, INW])
    pre = pb.dout("pre", [1024, INW])
    if prologue:
        p0 = pb.din("p0", [1024, D])
        p1 = pb.din("p1", [1024, D])
        g2d = pb.din("g2", [128, D])
        xo = pb.dout("xo", [1024, D])
        g2 = pb.sb("g2s", [128, D])
        pb.dma("sync", g2[:], g2d[:], [g2d], g2)
    ident = make_ident(pb, BF16)
    onepsc = pb.sb("onepsc", [128, D])
    shs = pb.sb("shs", [128, D])
    pb.dma("sync", onepsc[:], sc[:], [sc], onepsc)
    pb.dma("sync", shs[:], shd[:], [shd], shs)
    pb.op("vector", lambda e: e.tensor_scalar(out=onepsc[:], in0=onepsc[:], scalar1=1.0, scalar2=None, op0=ALU.add),
          [onepsc], [onepsc])
    hT = pb.sb("hT", [128, 16, 1024], BF16)
    xts = [pb.sb("xt%d" % i, [128, D]) for i in range(2)]
    pts = [pb.sb("pt%d" % i, [128, D]) for i in range(2)] if prologue else None
    junk = pb.sb("junk", [128, D])
    hb = pb.sb("hb", [128, D], BF16)
    ssq = pb.sb("ssq", [128, 1])
    ptr = pb.banks()[4:6]
    for t in range(NT):
        xt = xts[t % 2]
        pb.dma("sync", xt[:], x[t * 128:(t + 1) * 128, :], [x], xt)
        if prologue:
            pt = pts[t % 2]
            pb.dma("sync", pt[:], p0[t * 128:(t + 1) * 128, :], [p0], pt)
            pb.dma("sync", junk[:], p1[t * 128:(t + 1) * 128, :], [p1], junk)
            pb.op("vector", lambda e, pt=pt: e.tensor_tensor(out=pt[:], in0=pt[:], in1=junk[:], op=ALU.add), [pt, junk], [pt])
            pb.op("vector", lambda e, pt=pt: e.tensor_tensor(out=pt[:], in0=pt[:], in1=g2[:], op=ALU.mult), [pt, g2], [pt])
            pb.op("vector", lambda e, pt=pt, xt=xt: e.tensor_tensor(out=xt[:], in0=xt[:], in1=pt[:], op=ALU.add), [pt, xt], [xt])
            pb.dma("sync", xo[t * 128:(t + 1) * 128, :], xt[:], [xt], xo)
        emit_norm_mod(pb, xt, onepsc, shs, junk, ssq, hb)
        for g in range(4):
            pt_ = ptr[g % 2]
            for q in range(4):
                k = g * 4 + q
                pb.mm(pt_[:, q * 128:(q + 1) * 128], hb[:, k * 128:(k + 1) * 128], ident[:], True, True, [hb, ident], [pt_])
            pb.act(None, hT[:, g * 4:(g + 1) * 4, t * 128:(t + 1) * 128],
                   pt_[:].rearrange("p (q n) -> p q n", q=4), AF.Copy, [pt_], [hT])
    wv = w.t.rearrange("(k p) n -> p k n", p=128)
    wts = [pb.sb("w%d" % i, [128, 16, 512], BF16) for i in range(2)]
    pss = pb.banks()[0:4]
    stg = [pb.sb("stg%d" % i, [128, 512]) for i in range(4)]
    cnt = 0
    for j in range(10):
        c0 = j * 512
        cw = min(512, INW - c0)
        wt = wts[j % 2]
        pb.dma("gpsimd", wt[:, :, :cw], wv[:, :, c0:c0 + cw], [w], wt)
        for t in range(NT):
            pst = pss[cnt % 4]
            st = stg[cnt % 4]
            for k in range(16):
                pb.mm(pst[:, :cw], hT[:, k, t * 128:(t + 1) * 128], wt[:, k, :cw], k == 0, k == 15, [hT, wt], [pst])
            if cnt % 2 == 0:
                pb.op("vector", lambda e, st=st, pst=pst, cw=cw: e.tensor_copy(out=st[:, :cw], in_=pst[:, :cw]), [pst], [st])
            else:
                pb.act(None, st[:, :cw], pst[:, :cw], AF.Copy, [pst], [st])
            pb.dma("sync", pre[t * 128:(t + 1) * 128, c0:c0 + cw], st[:, :cw], [st], pre)
            cnt += 1
    return pb.finish() if own else None


def run_L1(nc, xs, mod_l, w_in_l, prologue=None):
    maps = []
    for i in range(8):
        b, r = i // 2, i % 2
        sl = slice(r * 1024, (r + 1) * 1024)
        m = {"x": np.ascontiguousarray(xs[b, sl]), "sc": rep128(mod_l[b, D:2 * D]), "sh": rep128(mod_l[b, 0:D]),
             "w": w_in_l}
        if prologue is not None:
            pp, g2 = prologue
            m["p0"] = np.ascontiguousarray(pp[2 * b][sl])
            m["p1"] = np.ascontiguousarray(pp[2 * b + 1][sl])
            m["g2"] = rep128(g2[b])
        maps.append(m)
    res = run(nc, maps)
    pre = np.zeros((4, S, INW), np.float32)
    xo = np.zeros((4, S, D), np.float32) if prologue is not None else None
    for i in range(8):
        b, r = i // 2, i % 2
        pre[b, r * 1024:(r + 1) * 1024] = res[i]["pre"]
        if prologue is not None:
            xo[b, r * 1024:(r + 1) * 1024] = res[i]["xo"]
    return pre, xo


def build_L2(pb=None):
    own = pb is None
    pb = pb or PB()
    NT = 16
    if "aq" in pb.bind:
        aqd, akd = pb.bind["aq"], pb.bind["ak"]
    else:
        qk = pb.din("qk", [S, 640])
        aqd, akd = T(qk.t[:, 0:512], qk.b), T(qk.t[:, 512:640], qk.b)
    csd = pb.din("cs", [S, 640])
    snd = pb.din("sn", [S, 640])
    gaind = pb.din("gain", [128, 640])
    avd = pb.din("av", [S, 128])
    sinkd = pb.din("sinkr", [128, 512])
    mqd = pb.din("mq", [S, 256])
    mkd = pb.din("mk", [S, 256])
    mvd = pb.din("mv", [S, 512])
    mod_ = pb.din("mo", [S, 512])
    gtd = pb.din("gt", [S, 8]) if "gt16" not in pb.bind else None
    bgd = pb.din("bg", [128, 128])
    mgd = pb.din("mgain", [128, 512])
    attT = pb.dout("attT", [4, 128, S], BF16)
    moT = pb.dout("moT", [4, 128, S], BF16)

    PS = pb.banks()
    ident = make_ident(pb, BF16)
    m_ge_f = tri_mask(pb, "mge", ALU.is_ge, F32)
    m_le_f = tri_mask(pb, "mle", ALU.is_le, F32)
    m_ge = pb.sb("mgeb", [128, 128], BF16)
    m_le = pb.sb("mleb", [128, 128], BF16)
    pb.op("vector", lambda e: e.tensor_copy(out=m_ge[:], in_=m_ge_f[:]), [m_ge_f], [m_ge])
    pb.op("vector", lambda e: e.tensor_copy(out=m_le[:], in_=m_le_f[:]), [m_le_f], [m_le])
    m_ge4 = pb.sb("mge4", [128, 4, 128], BF16)
    m_le4 = pb.sb("mle4", [128, 4, 128], BF16)
    for g in range(4):
        pb.op("vector", lambda e, g=g: e.tensor_copy(out=m_ge4[:, g, :], in_=m_ge_f[:]), [m_ge_f], [m_ge4])
        pb.op("vector", lambda e, g=g: e.tensor_copy(out=m_le4[:, g, :], in_=m_le_f[:]), [m_le_f], [m_le4])
    ones_f = pb.sb("ones_f", [128, 128])
    ones_b = pb.sb("ones_b", [128, 128], BF16)
    pb.op("gpsimd", lambda e: e.memset(ones_f[:], 1.0), [], [ones_f])
    pb.op("gpsimd", lambda e: e.memset(ones_b[:], 1.0), [], [ones_b])

    gain = pb.sb("gain", [128, 640])
    pb.dma("sync", gain[:], gaind[:], [gaind], gain)
    sinke = pb.sb("sinke", [128, 512])
    pb.dma("sync", sinke[:], sinkd[:], [sinkd], sinke)
    pb.act(None, sinke[:], sinke[:], AF.Exp, [sinke], [sinke])

    qT = pb.sb("qT", [128, NT, 512], BF16)
    kT = pb.sb("kT", [128, NT, 128], BF16)
    vb = pb.sb("vb", [128, NT, 128], BF16)
    attTs = pb.sb("attTs", [128, 4, S], BF16)

    qkt = [pb.sb("qkt%d" % i, [128, 640]) for i in range(2)]
    cst = [pb.sb("cst%d" % i, [128, 640]) for i in range(2)]
    snt = [pb.sb("snt%d" % i, [128, 640]) for i in range(2)]
    avt = [pb.sb("avt%d" % i, [128, 128]) for i in range(2)]
    junk = pb.sb("junk5", [128, 640])
    qn = pb.sb("qn", [128, 640])
    ra = pb.sb("ra", [128, 640])
    rb = pb.sb("rb", [128, 640])
    rot = pb.sb("rot", [128, 640], BF16)
    ss5 = pb.sb("ss5", [128, 5])
    for t in range(NT):
        x_ = qkt[t % 2]
        c_ = cst[t % 2]
        s_ = snt[t % 2]
        a_ = avt[t % 2]
        rs = slice(t * 128, (t + 1) * 128)
        pb.dma("sync", x_[:, 0:512], aqd[rs, :], [aqd], x_)
        pb.dma("sync", x_[:, 512:640], akd[rs, :], [akd], x_)
        pb.dma("sync", c_[:], csd[rs, :], [csd], c_)
        pb.dma("sync", s_[:], snd[rs, :], [snd], s_)
        pb.dma("sync", a_[:], avd[rs, :], [avd], a_)
        pb.act(None, junk[:], x_[:], AF.Square, [x_], [junk])
        pb.op("vector", lambda e: e.tensor_reduce(out=ss5[:], in_=junk[:].rearrange("p (h d) -> p h d", h=5),
                                                   axis=AX.X, op=ALU.add), [junk], [ss5])
        pb.act(None, ss5[:], ss5[:], AF.Sqrt, [ss5], [ss5], bias=EPS, scale=1.0 / 128)
        pb.op("vector", lambda e: e.reciprocal(out=ss5[:], in_=ss5[:]), [ss5], [ss5])
        for h in range(5):
            hs = slice(h * 128, (h + 1) * 128)
            pb.op("vector", lambda e, hs=hs, h=h, x_=x_: e.scalar_tensor_tensor(
                out=qn[:, hs], in0=x_[:, hs], scalar=ss5[:, h:h + 1], in1=gain[:, hs], op0=ALU.mult, op1=ALU.mult),
                [x_, ss5, gain], [qn])
        pb.op("gpsimd", lambda e, c_=c_: e.tensor_tensor(out=ra[:], in0=qn[:], in1=c_[:], op=ALU.mult), [qn, c_], [ra])
        qn4 = qn[:].rearrange("p (h t d) -> p h t d", h=5, t=2)
        rb4 = rb[:].rearrange("p (h t d) -> p h t d", h=5, t=2)
        sn4 = s_[:].rearrange("p (h t d) -> p h t d", h=5, t=2)
        pb.op("vector", lambda e, qn4=qn4, rb4=rb4, sn4=sn4: e.tensor_tensor(
            out=rb4[:, :, 0, :], in0=qn4[:, :, 1, :], in1=sn4[:, :, 0, :], op=ALU.mult), [qn, s_], [rb])
        pb.op("vector", lambda e, qn4=qn4, rb4=rb4, sn4=sn4: e.tensor_tensor(
            out=rb4[:, :, 1, :], in0=qn4[:, :, 0, :], in1=sn4[:, :, 1, :], op=ALU.mult), [qn, s_], [rb])
        pb.op("gpsimd", lambda e: e.tensor_tensor(out=rot[:], in0=ra[:], in1=rb[:], op=ALU.add), [ra, rb], [rot])
        pq = PS[t % 2]
        pk = PS[2 + t % 2]
        for h in range(4):
            pb.mm(pq[:, h * 128:(h + 1) * 128], rot[:, h * 128:(h + 1) * 128], ident[:], True, True, [rot, ident], [pq])
        pb.mm(pk[:, 0:128], rot[:, 512:640], ident[:], True, True, [rot, ident], [pk])
        pb.act(None, qT[:, t, :], pq[:], AF.Copy, [pq], [qT])
        pb.op("vector", lambda e, pk=pk, t=t: e.tensor_copy(out=kT[:, t, :], in_=pk[:, 0:128]), [pk], [kT])
        pb.op("gpsimd", lambda e, a_=a_, t=t: e.tensor_copy(out=vb[:, t, :], in_=a_[:]), [a_], [vb])

    Es = [pb.sb("E%d" % i, [128, 512], BF16) for i in range(4)]
    dens = [pb.sb("den%d" % i, [128, 512]) for i in range(2)]
    ec = 0
    for n in range(NT):
        po = PS[4 + n % 2]
        pd = PS[6 + n % 2]
        kbs = [kb for kb in (n - 1, n, n + 1) if 0 <= kb < NT]
        for i, kb in enumerate(kbs):
            psc = PS[ec % 4]
            E = Es[ec % 4]
            ec += 1
            pb.mm(psc[:], kT[:, kb, :], qT[:, n, :], True, True, [kT, qT], [psc])
            pb.act(None, E[:], psc[:], AF.Exp, [psc], [E], scale=1.0 / math.sqrt(128.0))
            if kb != n:
                mk4 = m_ge4 if kb < n else m_le4
                pb.op("gpsimd", lambda e, E=E, mk4=mk4: e.tensor_tensor(
                    out=E[:], in0=E[:], in1=mk4[:].rearrange("p g q -> p (g q)"), op=ALU.mult), [E, mk4], [E])
            pb.mm(po[:], vb[:, kb, :], E[:], i == 0, i == len(kbs) - 1, [vb, E], [po])
            pb.mm(pd[:], ones_b[:], E[:], i == 0, i == len(kbs) - 1, [ones_b, E], [pd])
        dn = dens[n % 2]
        pb.op("vector", lambda e, dn=dn, pd=pd: e.tensor_tensor(out=dn[:], in0=pd[:], in1=sinke[:], op=ALU.add), [pd, sinke], [dn])
        pb.op("vector", lambda e, dn=dn: e.reciprocal(out=dn[:], in_=dn[:]), [dn], [dn])
        pb.op("vector", lambda e, dn=dn, po=po, n=n: e.tensor_tensor(
            out=attTs[:, :, n * 128:(n + 1) * 128], in0=po[:].rearrange("p (g q) -> p g q", g=4),
            in1=dn[:].rearrange("p (g q) -> p g q", g=4), op=ALU.mult), [po, dn], [attTs])
    for g in range(4):
        pb.dma("sync", attT[g], attTs[:, g, :], [attTs], attT)
    if "mixo" in pb.bind:
        mixo_, mixg_ = pb.bind["mixo"], pb.bind["mixg"]
        coll_gather(pb, T(mixo_.t.rearrange("c p t -> (c p) t")[0:512, :], mixo_.b),
                    T(mixg_[0].t.rearrange("q c p t -> (q c p) t"), mixg_[0].b))

    mq = pb.sb("mq", [128, NT, 256])
    mk = pb.sb("mk", [128, NT, 256])
    v1 = pb.sb("v1", [128, NT, 2, 257], BF16)
    hacc = pb.sb("hacc", [128, NT, 512])
    gt = pb.sb("gt", [128, NT, 8])
    bg = pb.sb("bg", [128, NT, 8])
    pb.dma("sync", mq[:], mqd.t.rearrange("(t p) c -> p t c", p=128), [mqd], mq)
    pb.dma("sync", mk[:], mkd.t.rearrange("(t p) c -> p t c", p=128), [mkd], mk)
    if gtd is not None:
        pb.dma("sync", gt[:], gtd.t.rearrange("(t p) c -> p t c", p=128), [gtd], gt)
    else:
        g16d, rsel = pb.bind["gt16"], pb.bind["rsel"]
        gt16 = pb.sb("gt16", [128, NT, 16])
        pb.dma("sync", gt16[:], g16d.t.rearrange("(t p) c -> p t c", p=128), [g16d], gt16)
        pb.op("vector", lambda e: e.tensor_copy(
            out=gt[:].rearrange("p t (g h) -> p t g h", g=4),
            in_=gt16[:].rearrange("p t (g h) -> p t g h", g=4)[:, :, :, 2 * rsel:2 * rsel + 2]), [gt16], [gt])
    pb.dma("sync", bg[:], bgd.t.rearrange("p (t c) -> p t c", c=8), [bgd], bg)
    pb.op("gpsimd", lambda e: e.memset(v1[:, :, :, 256:257], 1.0), [], [v1])
    mvt = [pb.sb("mvt%d" % i, [128, 512]) for i in range(2)]
    for t in range(NT):
        m_ = mvt[t % 2]
        pb.dma("sync", m_[:], mvd[t * 128:(t + 1) * 128, :], [mvd], m_)
        pb.op("gpsimd", lambda e, m_=m_, t=t: e.tensor_copy(out=v1[:, t, :, 0:256],
                                                             in_=m_[:].rearrange("p (h c) -> p h c", h=2)), [m_], [v1])
    pb.op("vector", lambda e: e.tensor_tensor(out=gt[:], in0=gt[:], in1=bg[:], op=ALU.add), [gt, bg], [gt])
    logf = pb.sb("logf", [128, NT, 4])
    pb.act(None, logf[:], gt[:, :, 4:8], AF.Exp, [gt], [logf], scale=-1.0)
    pb.act(None, logf[:], logf[:], AF.Ln, [logf], [logf], bias=1.0, scale=1.0)
    pb.op("vector", lambda e: e.tensor_scalar(out=logf[:], in0=logf[:], scalar1=-1.0, scalar2=None, op0=ALU.mult), [logf], [logf])
    pc = PS[0]
    pcv = pc[:, 0:128].rearrange("p (t c) -> p t c", c=8)
    for t in range(NT):
        pb.mm(pcv[:, t, 0:2], m_le_f[:], logf[:, t, 0:2], True, True, [m_le_f, logf], [pc])
        pb.mm(pcv[:, t, 2:4], m_ge_f[:], logf[:, t, 2:4], True, True, [m_ge_f, logf], [pc])
        pb.mm(pcv[:, t, 4:8], ones_f[:], logf[:, t, 0:4], True, True, [ones_f, logf], [pc])
    args = pb.sb("args", [128, NT, 16])
    pb.op("vector", lambda e: e.tensor_copy(out=args[:, :, 0:4], in_=pcv[:, :, 0:4]), [pc], [args])
    pb.op("vector", lambda e: e.tensor_tensor(out=args[:, :, 4:8], in0=gt[:, :, 0:4], in1=args[:, :, 0:4], op=ALU.subtract), [gt, args], [args])
    pb.op("vector", lambda e: e.tensor_tensor(out=args[:, :, 8:12], in0=args[:, :, 4:8], in1=pcv[:, :, 4:8], op=ALU.add), [pc, args], [args])
    pb.op("vector", lambda e: e.tensor_copy(out=args[:, :, 12:16], in_=pcv[:, :, 4:8]), [pc], [args])
    E16 = pb.sb("E16", [128, NT, 16])
    pb.act(None, E16[:], args[:], AF.Exp, [args], [E16])

    Sst = [pb.sb("Sst%d" % i, [128, 257]) for i in range(2)]
    Sbf = [pb.sb("Sbf%d" % i, [128, 257], BF16) for i in range(2)]
    NBUF = 1
    qs = [pb.sb("qs%d" % i, [128, 128], BF16) for i in range(2 * NBUF)]
    ks = [pb.sb("ks%d" % i, [128, 128], BF16) for i in range(2 * NBUF)]
    kst = [pb.sb("kst%d" % i, [128, 128], BF16) for i in range(2 * NBUF)]
    qkTs = [pb.sb("qkT%d" % i, [128, 256], BF16) for i in range(2 * NBUF)]
    smT = [pb.sb("smT%d" % i, [128, 128], BF16) for i in range(2 * NBUF)]
    dnm = [pb.sb("dnm%d" % i, [128, 1]) for i in range(2 * NBUF)]
    DK = 128.0 ** -0.5
    for dr in range(2):
        msk = m_le_f if dr == 0 else m_ge_f
        for hh in range(2):
            pb.op("gpsimd", lambda e, hh=hh: e.memset(Sst[hh][:], 0.0), [], [Sst[hh]])
            pb.op("gpsimd", lambda e, hh=hh: e.memset(Sbf[hh][:], 0.0), [], [Sbf[hh]])
        for ci in range(NT):
            t = ci if dr == 0 else NT - 1 - ci
            for hh in range(2):
                col = dr * 2 + hh
                hs = slice(hh * 128, (hh + 1) * 128)
                bi = hh * NBUF + ci % NBUF
                q_, k_, ks_, qkT_, sm_, dn_ = qs[bi], ks[bi], kst[bi], qkTs[bi], smT[bi], dnm[bi]
                ptr = PS[hh * 4 + 0]
                psm = PS[hh * 4 + 0]
                pso = PS[hh * 4 + 1 + ci % 2]
                pkv = PS[hh * 4 + 3]
                pb.op("vector", lambda e, q_=q_, t=t, hs=hs, col=col: e.tensor_scalar(
                    out=q_[:], in0=mq[:, t, hs], scalar1=E16[:, t, col:col + 1], scalar2=DK, op0=ALU.mult, op1=ALU.mult),
                    [mq, E16], [q_])
                pb.act(None, k_[:], mk[:, t, hs], AF.Copy, [mk, E16], [k_], scale=E16[:, t, 4 + col:5 + col])
                pb.act(None, ks_[:], mk[:, t, hs], AF.Copy, [mk, E16], [ks_], scale=E16[:, t, 8 + col:9 + col])
                pb.mm(ptr[:, 0:128], q_[:], ident[:], True, True, [q_, ident], [ptr])
                pb.mm(ptr[:, 128:256], k_[:], ident[:], True, True, [k_, ident], [ptr])
                pb.op("vector", lambda e, qkT_=qkT_, ptr=ptr: e.tensor_copy(out=qkT_[:], in_=ptr[:, 0:256]), [ptr], [qkT_])
                pb.mm(psm[:, 256:384], qkT_[:, 128:256], qkT_[:, 0:128], True, True, [qkT_], [psm])
                pb.op("vector", lambda e, sm_=sm_, psm=psm, msk=msk: e.tensor_tensor(
                    out=sm_[:], in0=psm[:, 256:384], in1=msk[:], op=ALU.mult), [psm, msk], [sm_])
                pb.mm(pso[:, 0:257], sm_[:], v1[:, t, hh, :], True, False, [sm_, v1], [pso])
                pb.mm(pso[:, 0:257], qkT_[:, 0:128], Sbf[hh][:], False, True, [qkT_, Sbf[hh]], [pso])
                pb.act(None, dn_[:], pso[:, 256:257], AF.Abs, [pso], [dn_])
                pb.op("vector", lambda e, dn_=dn_: e.tensor_scalar(
                    out=dn_[:], in0=dn_[:], scalar1=1.0, scalar2=None, op0=ALU.max), [dn_], [dn_])
                pb.op("vector", lambda e, dn_=dn_: e.reciprocal(out=dn_[:], in_=dn_[:]), [dn_], [dn_])
                hsl = slice(hh * 256, (hh + 1) * 256)
                if dr == 0:
                    pb.op("vector", lambda e, dn_=dn_, pso=pso, t=t, hsl=hsl: e.tensor_scalar(
                        out=hacc[:, t, hsl], in0=pso[:, 0:256], scalar1=dn_[:, 0:1], scalar2=None, op0=ALU.mult),
                        [pso, dn_], [hacc])
                else:
                    pb.op("vector", lambda e, dn_=dn_, pso=pso, t=t, hsl=hsl: e.scalar_tensor_tensor(
                        out=hacc[:, t, hsl], in0=pso[:, 0:256], scalar=dn_[:, 0:1], in1=hacc[:, t, hsl],
                        op0=ALU.mult, op1=ALU.add), [pso, dn_, hacc], [hacc])
                pb.mm(pkv[:, 0:257], ks_[:], v1[:, t, hh, :], True, True, [ks_, v1], [pkv])
                pb.op("vector", lambda e, hh=hh, pkv=pkv, t=t, col=col: e.scalar_tensor_tensor(
                    out=Sst[hh][:], in0=Sst[hh][:], scalar=E16[:, t, 12 + col:13 + col], in1=pkv[:, 0:257],
                    op0=ALU.mult, op1=ALU.add), [Sst[hh], E16, pkv], [Sst[hh]])
                pb.act(None, Sbf[hh][:], Sst[hh][:], AF.Copy, [Sst[hh]], [Sbf[hh]])

    mgain = pb.sb("mgain", [128, 512])
    pb.dma("sync", mgain[:], mgd[:], [mgd], mgain)
    moTs = pb.sb("moTs", [128, 4, S], BF16)
    mot = [pb.sb("mot%d" % i, [128, 512]) for i in range(2)]
    hn = pb.sb("hn", [128, 512])
    mob = pb.sb("mob", [128, 512], BF16)
    jk = pb.sb("jk", [128, 256])
    ss2 = pb.sb("ss2", [128, 2])
    for t in range(NT):
        o_ = mot[t % 2]
        pb.dma("sync", o_[:], mod_[t * 128:(t + 1) * 128, :], [mod_], o_)
        pb.act(None, o_[:], o_[:], AF.Sigmoid, [o_], [o_])
        for hh in range(2):
            pb.act(None, jk[:], hacc[:, t, hh * 256:(hh + 1) * 256], AF.Square, [hacc], [jk, ss2], accum_out=ss2[:, hh:hh + 1])
        pb.act(None, ss2[:], ss2[:], AF.Sqrt, [ss2], [ss2], bias=EPS, scale=1.0 / 256)
        pb.op("vector", lambda e: e.reciprocal(out=ss2[:], in_=ss2[:]), [ss2], [ss2])
        for hh in range(2):
            hsl = slice(hh * 256, (hh + 1) * 256)
            pb.op("vector", lambda e, hh=hh, hsl=hsl, t=t: e.scalar_tensor_tensor(
                out=hn[:, hsl], in0=hacc[:, t, hsl], scalar=ss2[:, hh:hh + 1], in1=mgain[:, hsl],
                op0=ALU.mult, op1=ALU.mult), [hacc, ss2, mgain], [hn])
        pb.op("gpsimd", lambda e, o_=o_: e.tensor_tensor(out=mob[:], in0=hn[:], in1=o_[:], op=ALU.mult), [hn, o_], [mob])
        pp = PS[t % 2]
        for j in range(4):
            pb.mm(pp[:, j * 128:(j + 1) * 128], mob[:, j * 128:(j + 1) * 128], ident[:], True, True, [mob, ident], [pp])
        pb.act(None, moTs[:, :, t * 128:(t + 1) * 128], pp[:].rearrange("p (j q) -> p j q", j=4), AF.Copy, [pp], [moTs])
    for j in range(4):
        pb.dma("sync", moT[j], moTs[:, j, :], [moTs], moT)
    if "mixo" in pb.bind:
        coll_gather(pb, T(mixo_.t.rearrange("c p t -> (c p) t")[512:1024, :], mixo_.b),
                    T(mixg_[1].t.rearrange("q c p t -> (q c p) t"), mixg_[1].b))
    return pb.finish() if own else None


def rope_tables_np():
    inv = 1.0 / (10000.0 ** (np.arange(0, 128, 2, dtype=np.float32) / 128))
    ang = np.arange(S, dtype=np.float32)[:, None] * inv[None, :]
    ang = np.concatenate([ang, ang], axis=-1)
    cos = np.cos(ang).astype(np.float32)
    sin = np.sin(ang).astype(np.float32)
    sinm = np.concatenate([-sin[:, :64], sin[:, 64:]], axis=-1)
    return np.ascontiguousarray(np.tile(cos, (1, 5))), np.ascontiguousarray(np.tile(sinm, (1, 5)))


def run_L2(nc, pre, b_gates_l, q_gain_l, k_gain_l, sink_l, m_gain_l):
    cs5, sn5 = rope_tables_np()
    maps = []
    for i in range(8):
        b, r = i // 2, i % 2
        p = pre[b]
        aq = p[:, r * 512:(r + 1) * 512]
        ak = p[:, 1024 + r * 128:1024 + (r + 1) * 128]
        av = p[:, 1280 + r * 128:1280 + (r + 1) * 128]
        mq = p[:, 1536 + r * 256:1536 + (r + 1) * 256]
        mk = p[:, 2048 + r * 256:2048 + (r + 1) * 256]
        mv = p[:, 2560 + r * 512:2560 + (r + 1) * 512]
        mo = p[:, 3584 + r * 512:3584 + (r + 1) * 512]
        mg = p[:, 4608:4624].reshape(S, 4, 4)[:, :, 2 * r:2 * r + 2].reshape(S, 8)
        bg = b_gates_l[:, 2 * r:2 * r + 2].reshape(8)
        maps.append({
            "qk": np.ascontiguousarray(np.concatenate([aq, ak], axis=1)),
            "cs": cs5, "sn": sn5,
            "gain": rep128(np.concatenate([np.tile(q_gain_l, 4), k_gain_l])),
            "av": np.ascontiguousarray(av),
            "sinkr": rep128(np.repeat(sink_l[4 * r:4 * r + 4], 128)),
            "mq": np.ascontiguousarray(mq), "mk": np.ascontiguousarray(mk), "mv": np.ascontiguousarray(mv),
            "mo": np.ascontiguousarray(mo), "gt": np.ascontiguousarray(mg),
            "bg": rep128(np.tile(bg, 16)),
            "mgain": rep128(m_gain_l[r * 512:(r + 1) * 512]),
        })
    res = run(nc, maps)
    mixT = np.zeros((4, 16, 128, S), NPBF)
    for i in range(8):
        b, r = i // 2, i % 2
        mixT[b, 4 * r:4 * r + 4] = res[i]["attT"]
        mixT[b, 8 + 4 * r:8 + 4 * r + 4] = res[i]["moT"]
    return mixT


def build_L3(pb=None):
    own = pb is None
    pb = pb or PB()
    NT = 8
    mixd = pb.din("mixT", [16, 128, 1024], BF16) if "mixg" not in pb.bind else None
    x = pb.din("x", [1024, D])
    w = pb.din("w", [D, D])
    g1d = pb.din("g1", [128, D])
    scd = pb.din("sc", [128, D])
    shd = pb.din("sh", [128, D])
    wrd = pb.din("wr", [D, NE])
    x1o = pb.dout("x1", [1024, D])
    h2o = pb.dout("h2", [1024, D], BF16)
    affo = pb.dout("aff", [1024, NE])
    affTo = pb.dout("affT", [NE, 1024])
    PS = pb.banks()
    identf = make_ident(pb, F32)
    mixT = pb.sb("mixT", [128, 16, 1024], BF16)
    if "mixg" in pb.bind:
        mixg, selv = pb.bind["mixg"], pb.bind["selv"]
        sel = pb.sb("sel", [128, 2])
        pb.dma("sync", sel[:], selv[:], [selv], sel)
        mtmp = [pb.sb("mtmp%d" % i, [128, 4, S], BF16) for i in range(1)]
        for qc in range(4):
            mt = mtmp[0]
            q, c0 = qc // 2, (qc % 2) * 4
            ks = slice(qc * 4, qc * 4 + 4)
            pb.dma("sync", mt[:], mixg[qc % 2].t[q].rearrange("c p t -> p c t"), [mixg[qc % 2]], mt)
            pb.op("vector", lambda e, ks=ks, mt=mt: e.tensor_scalar(
                out=mixT[:, ks, :], in0=mt[:, :, 0:1024], scalar1=sel[:, 0:1], scalar2=None, op0=ALU.mult),
                [mt, sel], [mixT])
            pb.op("vector", lambda e, ks=ks, mt=mt: e.scalar_tensor_tensor(
                out=mixT[:, ks, :], in0=mt[:, :, 1024:2048], scalar=sel[:, 1:2],
                in1=mixT[:, ks, :], op0=ALU.mult, op1=ALU.add), [mt, sel, mixT], [mixT])
        wbs = [pb.sb("wbk%d" % i, [128, 1, D], BF16) for i in range(16)]
        for kk in range(16):
            q, c8 = kk // 8, kk % 8
            g = 4 * q + c8 if c8 < 4 else 8 + 4 * q + (c8 - 4)
            pb.dma("gpsimd", wbs[kk][:, 0, :], w[g * 128:(g + 1) * 128, :], [w], wbs[kk])
        wsel = lambda k: wbs[k]
        widx = lambda k: 0
    else:
        wb = pb.sb("wb", [128, 16, D], BF16)
        for c in range(16):
            pb.dma("sync", mixT[:, c, :], mixd[c], [mixd], mixT)
        wv = w.t.rearrange("(k p) n -> p k n", p=128)
        for j in range(4):
            pb.dma("gpsimd", wb[:, :, j * 512:(j + 1) * 512], wv[:, :, j * 512:(j + 1) * 512], [w], wb)
        wsel = lambda k: wb
        widx = lambda k: k
    g1 = pb.sb("g1", [128, D])
    onepsc = pb.sb("onepsc", [128, D])
    shs = pb.sb("shs", [128, D])
    wr = pb.sb("wr", [128, 16, NE])
    pb.dma("sync", g1[:], g1d[:], [g1d], g1)
    pb.dma("sync", onepsc[:], scd[:], [scd], onepsc)
    pb.dma("sync", shs[:], shd[:], [shd], shs)
    pb.dma("sync", wr[:], wrd.t.rearrange("(k p) e -> p k e", p=128), [wrd], wr)
    pb.op("vector", lambda e: e.tensor_scalar(out=onepsc[:], in0=onepsc[:], scalar1=1.0, scalar2=None, op0=ALU.add),
          [onepsc], [onepsc])
    xts = [pb.sb("xt%d" % i, [128, D]) for i in range(2)]
    junk = pb.sb("junk", [128, D])
    h2f = pb.sb("h2f", [128, D])
    h2b = [pb.sb("h2b%d" % i, [128, D], BF16) for i in range(2)]
    h2T = pb.sb("h2T", [128, 16, 128])
    ssq = pb.sb("ssq", [128, 1])
    lg = pb.sb("lg", [128, NE])
    mx = pb.sb("mx", [128, 1])
    sm = pb.sb("sm", [128, 1])
    affs = [pb.sb("affs%d" % i, [128, NE]) for i in range(2)]
    affTs = pb.sb("affTs", [NE, 1024])
    for t in range(NT):
        xt = xts[t % 2]
        ts_ = slice(t * 128, (t + 1) * 128)
        pb.dma("sync", xt[:], x[ts_, :], [x], xt)
        for j in range(4):
            pst = PS[j]
            for k in range(16):
                pb.mm(pst[:], mixT[:, k, ts_], wsel(k)[:, widx(k), j * 512:(j + 1) * 512], k == 0, k == 15, [mixT, wsel(k)], [pst])
            js = slice(j * 512, (j + 1) * 512)
            pb.op("vector", lambda e, pst=pst, js=js: e.tensor_tensor(out=junk[:, js], in0=pst[:], in1=g1[:, js], op=ALU.mult),
                  [pst, g1], [junk])
        pb.op("gpsimd", lambda e, xt=xt: e.tensor_tensor(out=xt[:], in0=xt[:], in1=junk[:], op=ALU.add), [xt, junk], [xt])
        pb.dma("sync", x1o[ts_, :], xt[:], [xt], x1o)
        emit_norm_mod(pb, xt, onepsc, shs, junk, ssq, h2f)
        hb = h2b[t % 2]
        pb.act(None, hb[:], h2f[:], AF.Copy, [h2f], [hb])
        pb.dma("sync", h2o[ts_, :], hb[:], [hb], h2o)
        for g in range(4):
            pt_ = PS[4 + g % 2]
            for q in range(4):
                k = g * 4 + q
                pb.mm(pt_[:, q * 128:(q + 1) * 128], h2f[:, k * 128:(k + 1) * 128], identf[:], True, True, [h2f, identf], [pt_])
            pb.op("vector", lambda e, pt_=pt_, g=g: e.tensor_copy(out=h2T[:, g * 4:(g + 1) * 4, :],
                                                                 in_=pt_[:].rearrange("p (q n) -> p q n", q=4)), [pt_], [h2T])
        pl = PS[6]
        for k in range(16):
            pb.mm(pl[:, 0:NE], h2T[:, k, :], wr[:, k, :], k == 0, k == 15, [h2T, wr], [pl])
        pb.op("vector", lambda e, pl=pl: e.tensor_copy(out=lg[:], in_=pl[:, 0:NE]), [pl], [lg])
        pb.op("vector", lambda e: e.tensor_reduce(out=mx[:], in_=lg[:], axis=AX.X, op=ALU.max), [lg], [mx])
        pb.op("vector", lambda e: e.tensor_scalar(out=mx[:], in0=mx[:], scalar1=-1.0, scalar2=None, op0=ALU.mult), [mx], [mx])
        af = affs[t % 2]
        pb.act(None, af[:], lg[:], AF.Exp, [lg, mx], [af, sm], bias=mx[:, 0:1], scale=1.0, accum_out=sm[:])
        pb.op("vector", lambda e: e.reciprocal(out=sm[:], in_=sm[:]), [sm], [sm])
        pb.op("vector", lambda e, af=af: e.tensor_scalar(out=af[:], in0=af[:], scalar1=sm[:, 0:1], scalar2=None, op0=ALU.mult),
              [af, sm], [af])
        pb.dma("sync", affo[ts_, :], af[:], [af], affo)
        pb.mm(PS[7][0:NE, 0:128], af[:], identf[:], True, True, [af, identf], [PS[7]])
        pb.op("vector", lambda e, ts_=ts_: e.tensor_copy(out=affTs[:, ts_], in_=PS[7][0:NE, 0:128]), [PS[7]], [affTs])
    pb.dma("sync", affTo[:], affTs[:], [affTs], affTo)
    return pb.finish() if own else None


def run_L3(nc, mixT, xs, mod_l, w_out_l, w_router_l):
    maps = []
    for i in range(8):
        b, r = i // 2, i % 2
        sl = slice(r * 1024, (r + 1) * 1024)
        maps.append({"mixT": np.ascontiguousarray(mixT[b][:, :, sl]), "x": np.ascontiguousarray(xs[b, sl]),
                     "w": w_out_l, "g1": rep128(mod_l[b, 2 * D:3 * D]), "sc": rep128(mod_l[b, 4 * D:5 * D]),
                     "sh": rep128(mod_l[b, 3 * D:4 * D]), "wr": w_router_l})
    res = run(nc, maps)
    x1 = np.zeros((4, S, D), np.float32)
    h2 = np.zeros((4, S, D), NPBF)
    aff = np.zeros((4, S, NE), np.float32)
    for i in range(8):
        b, r = i // 2, i % 2
        sl = slice(r * 1024, (r + 1) * 1024)
        x1[b, sl] = res[i]["x1"]
        h2[b, sl] = res[i]["h2"]
        aff[b, sl] = res[i]["aff"]
    return x1, h2, aff


def build_L4(pb=None):
    own = pb is None
    pb = pb or PB()
    NJ = 16
    NEL = 8
    h2d = pb.din("h2", [S, D], BF16) if "h2p" not in pb.bind else None
    affTd = pb.din("affT", [NEL, S])
    afftd = pb.din("afft", [S, NEL])
    wgd = pb.din("wg", [NEL, D, FF])
    wud = pb.din("wu", [NEL, D, FF])
    wdd = pb.din("wd", [NEL, FF, D])
    part = pb.dout("part", [S, D])
    yscr = pb.dscratch("yscr", [NEL * 2, 128, D], BF16)
    yscr.b.nowaw = True
    PS = pb.banks()
    identf = make_ident(pb, F32)
    identb = pb.sb("identb", [128, 128], BF16) if "rstate" not in pb.bind else pb.bind["rstate"]["identb"]
    pb.op("vector", lambda e: e.tensor_copy(out=identb[:], in_=identf[:]), [identf], [identb])
    ones_f = pb.sb("ones_f", [128, 128])
    ones_b = pb.sb("ones_b", [128, 128], BF16)
    pb.op("gpsimd", lambda e: e.memset(ones_f[:], 1.0), [], [ones_f])
    pb.op("gpsimd", lambda e: e.memset(ones_b[:], 1.0), [], [ones_b])
    usf = pb.sb("usf", [128, 128])
    usb = pb.sb("usb", [128, 128], BF16)
    pb.op("gpsimd", lambda e: e.memset(usf[:], 1.0), [], [usf])
    pb.op("gpsimd", lambda e: e.affine_select(out=usf[:], in_=usf[:], pattern=[[1, 128]], compare_op=ALU.is_ge,
                                                fill=0.0, base=-1, channel_multiplier=-1), [usf], [usf])
    pb.op("vector", lambda e: e.tensor_copy(out=usb[:], in_=usf[:]), [usf], [usb])
    RS = pb.bind.get("rstate")
    iota_i = pb.sb("iota_i", [128, 256], mybir.dt.int32)
    iota = pb.sb("iota", [128, 256]) if RS is None else RS["iota"]
    pb.op("gpsimd", lambda e: e.iota(iota_i[:], pattern=[[1, 256]], base=0, channel_multiplier=0), [], [iota_i])
    pb.op("vector", lambda e: e.tensor_copy(out=iota[:], in_=iota_i[:]), [iota_i], [iota])

    wg_t = [pb.sb("wg%d" % i, [128, 16, 256], BF16) for i in range(2)]
    wu_t = [pb.sb("wu%d" % i, [128, 16, 256], BF16) for i in range(2)]
    wd_t = [pb.sb("wd%d" % i, [128, 2, D], BF16) for i in range(4)]

    def load_gu(e, fp):
        if e >= NEL:
            return
        slot = fp % 2
        src_g = wgd.t[e].rearrange("(k p) f -> p k f", p=128)[:, :, fp * 256:(fp + 1) * 256]
        src_u = wud.t[e].rearrange("(k p) f -> p k f", p=128)[:, :, fp * 256:(fp + 1) * 256]
        pb.dma("gpsimd", wg_t[slot][:], src_g, [wgd], wg_t[slot])
        pb.dma("gpsimd", wu_t[slot][:], src_u, [wud], wu_t[slot])

    def load_d(e):
        if e >= NEL:
            return
        for fp in range(4):
            src = wdd.t[e].rearrange("(c p) d -> p c d", p=128)[:, fp * 2:(fp + 1) * 2, :]
            pb.dma("gpsimd", wd_t[fp][:], src, [wdd], wd_t[fp])

    h2s = pb.sb("h2s", [128, NJ, D], BF16)
    for j in range(NJ):
        if "h2p" in pb.bind:
            hp = pb.bind["h2p"][(j % 8) // 4]
            off = (j % 4) * 128
            pb.dma("sync", h2s[:, j, :], hp.t[j // 8, off:off + 128, :], [hp], h2s)
        else:
            pb.dma("sync", h2s[:, j, :], h2d[j * 128:(j + 1) * 128, :], [h2d], h2s)
    load_gu(0, 0)
    load_gu(0, 1)
    load_d(0)

    work = pb.sb("work", [NEL, S])
    m8 = pb.sb("m8", [NEL, 8])
    pb.dma("sync", work[:], affTd[:], [affTd], work)
    for it in range(CAP // 8):
        pb.op("vector", lambda e: e.max(out=m8[:], in_=work[:]), [work], [m8])
        if it < CAP // 8 - 1:
            pb.op("vector", lambda e: e.match_replace(out=work[:], in_to_replace=m8[:], in_values=work[:], imm_value=-1.0),
                  [work, m8], [work])
    diag8 = pb.sb("diag8", [NEL, NEL])
    pb.op("vector", lambda e: e.tensor_scalar(out=diag8[:], in0=identf[0:NEL, 0:NEL], scalar1=m8[:, 7:8], scalar2=None,
                                               op0=ALU.mult), [identf, m8], [diag8])
    pb.mm(PS[0][:, 0:NEL], ones_f[0:NEL, :], diag8[:], True, True, [ones_f, diag8], [PS[0]])
    thrB = pb.sb("thrB", [128, NEL])
    pb.op("vector", lambda e: e.tensor_copy(out=thrB[:], in_=PS[0][:, 0:NEL]), [PS[0]], [thrB])
    afft = pb.sb("afft", [128, NJ, NEL])
    pb.dma("sync", afft[:], afftd.t.rearrange("(j p) e -> p j e", p=128), [afftd], afft)
    mask = pb.sb("mask", [128, NJ, NEL])
    maskb = pb.sb("maskb", [128, NJ, NEL], BF16)
    gm = pb.sb("gm", [128, NJ, NEL]) if RS is None else RS["gm"]
    for j in range(NJ):
        pb.op("vector", lambda e, j=j: e.tensor_tensor(out=mask[:, j, :], in0=afft[:, j, :], in1=thrB[:], op=ALU.is_ge),
              [afft, thrB], [mask])
    pb.op("vector", lambda e: e.tensor_copy(out=maskb[:], in_=mask[:]), [mask], [maskb])
    pb.op("vector", lambda e: e.tensor_tensor(out=gm[:], in0=mask[:], in1=afft[:], op=ALU.mult), [mask, afft], [gm])
    ppos = PS[1]
    pposv = ppos[:, 0:NJ * NEL].rearrange("p (j e) -> p j e", e=NEL)
    for j in range(NJ):
        pb.mm(pposv[:, j, :], usb[:], maskb[:, j, :], True, j == 0, [usb, maskb], [ppos])
        for j2 in range(j):
            pb.mm(pposv[:, j, :], ones_b[:], maskb[:, j2, :], False, j2 == j - 1, [ones_b, maskb], [ppos])
    pos = pb.sb("pos", [128, NJ, NEL]) if RS is None else RS["pos"]
    pb.op("vector", lambda e: e.tensor_copy(out=pos[:], in_=pposv), [ppos], [pos])

    P01 = pb.sb("P01", [128, NJ, CAP], BF16)
    xeT = [pb.sb("xeT%d" % i, [128, 16, CAP], BF16) for i in range(2)]
    hidT = pb.sb("hidT", [128, 8, CAP], BF16)
    sil = [pb.sb("sil%d" % i, [128, CAP]) for i in range(2)]
    yst = [pb.sb("yst%d" % i, [128, D], BF16) for i in range(2)]
    ev = 0
    for e_ in range(NEL):
        xe = xeT[e_ % 2]
        for j in range(NJ):
            pb.op("vector", lambda e, j=j, e_=e_: e.tensor_scalar(
                out=P01[:, j, :], in0=iota[:], scalar1=pos[:, j, e_:e_ + 1], scalar2=mask[:, j, e_:e_ + 1],
                op0=ALU.is_equal, op1=ALU.mult), [iota, pos, mask], [P01])
        for dc in range(16):
            pg = PS[dc % 4]
            for j in range(NJ):
                pb.mm(pg[:, 0:CAP], h2s[:, j, dc * 128:(dc + 1) * 128], P01[:, j, :], j == 0, j == NJ - 1, [h2s, P01], [pg])
            if dc % 2 == 0:
                pb.act(None, xe[:, dc, :], pg[:, 0:CAP], AF.Copy, [pg], [xe])
            else:
                pb.op("vector", lambda e, xe=xe, pg=pg, dc=dc: e.tensor_copy(out=xe[:, dc, :], in_=pg[:, 0:CAP]), [pg], [xe])
        for fp in range(4):
            wg_ = wg_t[fp % 2]
            wu_ = wu_t[fp % 2]
            for f2 in range(2):
                fc = fp * 2 + f2
                pgu = PS[4 + fc % 2]
                for k in range(16):
                    pb.mm(pgu[:, 0:CAP], wg_[:, k, f2 * 128:(f2 + 1) * 128], xe[:, k, :], k == 0, k == 15, [wg_, xe], [pgu])
                for k in range(16):
                    pb.mm(pgu[:, CAP:2 * CAP], wu_[:, k, f2 * 128:(f2 + 1) * 128], xe[:, k, :], k == 0, k == 15, [wu_, xe], [pgu])
                sl_ = sil[fc % 2]
                pb.act(None, sl_[:], pgu[:, 0:CAP], AF.Silu, [pgu], [sl_])
                pb.op("vector", lambda e, sl_=sl_, pgu=pgu, fc=fc: e.tensor_tensor(
                    out=hidT[:, fc, :], in0=sl_[:], in1=pgu[:, CAP:2 * CAP], op=ALU.mult), [sl_, pgu], [hidT])
            nfp = fp + 2
            if nfp < 4:
                load_gu(e_, nfp)
            else:
                load_gu(e_ + 1, nfp - 4)
        for ct in range(2):
            ys = yst[ct]
            for dj in range(4):
                pd = PS[6 + dj % 2]
                for fc in range(8):
                    wd_ = wd_t[fc // 2]
                    pb.mm(pd[:], hidT[:, fc, ct * 128:(ct + 1) * 128], wd_[:, fc % 2, dj * 512:(dj + 1) * 512],
                          fc == 0, fc == 7, [hidT, wd_], [pd])
                if dj % 2 == 0:
                    pb.act(None, ys[:, dj * 512:(dj + 1) * 512], pd[:], AF.Copy, [pd], [ys])
                else:
                    pb.op("vector", lambda e, ys=ys, pd=pd, dj=dj: e.tensor_copy(out=ys[:, dj * 512:(dj + 1) * 512], in_=pd[:]), [pd], [ys])
            pb.dma("sync", yscr[e_ * 2 + ct], ys[:], [ys], yscr)
        load_d(e_ + 1)

    split = "psend" in pb.bind
    if split:
        bind_ = pb.bind
        pb.end()
        pb.begin(bind_)
        h2s = pb.sb("yes", [128, NJ, D], BF16)
        psend, pown, precv = bind_["psend"], bind_["pown"], bind_["precv"]
        sel = pb.sb("sel", [128, 2])
        osel = pb.sb("osel", [128, 2])
        pb.dma("sync", sel[:], bind_["selv"][:], [bind_["selv"]], sel)
        pb.dma("sync", osel[:], bind_["oselv"][:], [bind_["oselv"]], osel)
        outs2 = [[pb.sb("outt%d_%d" % (h, i), [128, D]) for i in range(2)] for h in range(2)]
        s1s = [pb.sb("s1_%d" % i, [128, D]) for i in range(2)]
        s2s = [pb.sb("s2_%d" % i, [128, D]) for i in range(2)]
    for i in range(NEL * 2):
        pb.dma("sync", h2s[:, i, :], yscr[i], [yscr], h2s)
    Pg = [pb.sb("Pg%d" % i, [128, CAP], BF16) for i in range(2)]
    PgT = [pb.sb("PgT%d" % i, [128, NEL, CAP], BF16) for i in range(2)]
    outt1 = pb.sb("outt", [128, D]) if not split else None
    c = 0
    order = [(jj, h) for jj in range(8) for h in range(2)] if split else [(j, None) for j in range(NJ)]
    for it, (jj, h) in enumerate(order):
        j = jj if h is None else h * 8 + jj
        outt = outt1 if h is None else outs2[h][jj % 2]
        pgt = PgT[it % 2]
        for e_ in range(NEL):
            pg_ = Pg[c % 2]
            ptp = PS[c % 4]
            c += 1
            pb.op("vector", lambda e, pg_=pg_, j=j, e_=e_: e.tensor_scalar(
                out=pg_[:], in0=iota[:], scalar1=pos[:, j, e_:e_ + 1], scalar2=gm[:, j, e_:e_ + 1],
                op0=ALU.is_equal, op1=ALU.mult), [iota, pos, gm], [pg_])
            for ct in range(2):
                pb.mm(ptp[:, ct * 128:(ct + 1) * 128], pg_[:, ct * 128:(ct + 1) * 128], identb[:], True, True, [pg_, identb], [ptp])
            pb.act(None, pgt[:, e_, :], ptp[:, 0:CAP], AF.Copy, [ptp], [pgt])
        for dj in range(4):
            po = PS[4 + dj]
            n = 0
            for e_ in range(NEL):
                for ct in range(2):
                    pb.mm(po[:], pgt[:, e_, ct * 128:(ct + 1) * 128], h2s[:, e_ * 2 + ct, dj * 512:(dj + 1) * 512],
                          n == 0, n == NEL * 2 - 1, [pgt, h2s], [po])
                    n += 1
            if dj % 2 == 0:
                pb.act(None, outt[:, dj * 512:(dj + 1) * 512], po[:], AF.Copy, [po], [outt])
            else:
                pb.op("vector", lambda e, po=po, dj=dj, outt=outt: e.tensor_copy(out=outt[:, dj * 512:(dj + 1) * 512], in_=po[:]), [po], [outt])
        if not split:
            pb.dma("sync", part[j * 128:(j + 1) * 128, :], outt[:], [outt], part)
        elif h == 1:
            A, B = outs2[0][jj % 2], outs2[1][jj % 2]
            s1, s2 = s1s[jj % 2], s2s[jj % 2]
            rows = slice(jj * 128, (jj + 1) * 128)
            pb.op("vector", lambda e, A=A, s1=s1: e.tensor_scalar(out=s1[:], in0=A[:], scalar1=osel[:, 0:1], scalar2=None,
                                                                  op0=ALU.mult), [A, osel], [s1])
            pb.op("vector", lambda e, B=B, s1=s1: e.scalar_tensor_tensor(out=s1[:], in0=B[:], scalar=osel[:, 1:2], in1=s1[:],
                                                                       op0=ALU.mult, op1=ALU.add), [B, osel, s1], [s1])
            pb.op("vector", lambda e, A=A, s2=s2: e.tensor_scalar(out=s2[:], in0=A[:], scalar1=sel[:, 0:1], scalar2=None,
                                                                  op0=ALU.mult), [A, sel], [s2])
            pb.op("vector", lambda e, B=B, s2=s2: e.scalar_tensor_tensor(out=s2[:], in0=B[:], scalar=sel[:, 1:2], in1=s2[:],
                                                                       op0=ALU.mult, op1=ALU.add), [B, sel, s2], [s2])
            pb.dma("sync", psend[rows, :], s1[:], [s1], psend)
            pb.dma("sync", pown[rows, :], s2[:], [s2], pown)
            if jj % 2 == 1:
                pc = jj // 2
                coll_gather(pb, T(psend.t[pc * 256:(pc + 1) * 256, :], psend.b),
                            T(precv[pc].t.rearrange("q t d -> (q t) d"), precv[pc].b))
    return pb.finish() if own else None


def run_L4(nc, h2, aff, w_gate_l, w_up_l, w_down_l):
    maps = []
    for i in range(8):
        b, r = i // 2, i % 2
        es = slice(8 * r, 8 * r + 8)
        maps.append({"h2": h2[b], "affT": np.ascontiguousarray(aff[b][:, es].T), "afft": np.ascontiguousarray(aff[b][:, es]),
                     "wg": w_gate_l[es], "wu": w_up_l[es], "wd": w_down_l[es]})
    res = run(nc, maps)
    return [res[i]["part"] for i in range(8)]


def build_L5(pb=None):
    own = pb is None
    pb = pb or PB()
    x = pb.din("x", [1024, D])
    p0 = pb.din("p0", [1024, D])
    p1 = pb.din("p1", [1024, D])
    g2d = pb.din("g2", [128, D])
    xo = pb.dout("xo", [1024, D])
    g2 = pb.sb("g2s", [128, D])
    pb.dma("sync", g2[:], g2d[:], [g2d], g2)
    xts = [pb.sb("xt%d" % i, [128, D]) for i in range(2)]
    pts = [pb.sb("pt%d" % i, [128, D]) for i in range(2)]
    qts = [pb.sb("qt%d" % i, [128, D]) for i in range(2)]
    for t in range(8):
        xt, pt, qt = xts[t % 2], pts[t % 2], qts[t % 2]
        ts_ = slice(t * 128, (t + 1) * 128)
        pb.dma("sync", xt[:], x[ts_, :], [x], xt)
        pb.dma("sync", pt[:], p0[ts_, :], [p0], pt)
        pb.dma("sync", qt[:], p1[ts_, :], [p1], qt)
        pb.op("vector", lambda e, pt=pt, qt=qt: e.tensor_tensor(out=pt[:], in0=pt[:], in1=qt[:], op=ALU.add), [pt, qt], [pt])
        pb.op("gpsimd", lambda e, pt=pt: e.tensor_tensor(out=pt[:], in0=pt[:], in1=g2[:], op=ALU.mult), [pt, g2], [pt])
        pb.op("vector", lambda e, pt=pt, xt=xt: e.tensor_tensor(out=xt[:], in0=xt[:], in1=pt[:], op=ALU.add), [pt, xt], [xt])
        pb.dma("sync", xo[ts_, :], xt[:], [xt], xo)
    return pb.finish() if own else None


def run_L5(nc, x1, parts, g2):
    maps = []
    for i in range(8):
        b, r = i // 2, i % 2
        sl = slice(r * 1024, (r + 1) * 1024)
        maps.append({"x": np.ascontiguousarray(x1[b, sl]), "p0": np.ascontiguousarray(parts[2 * b][sl]),
                     "p1": np.ascontiguousarray(parts[2 * b + 1][sl]), "g2": rep128(g2[b])})
    res = run(nc, maps)
    out = np.zeros((4, S, D), np.float32)
    for i in range(8):
        b, r = i // 2, i % 2
        out[b, r * 1024:(r + 1) * 1024] = res[i]["xo"]
    return out


def mod_steps(pb, cTd, wd_, brepd, modrep, nchunk, lbase, banks):
    ones_f = pb.sb("ones_f", [128, 128])
    pb.op("gpsimd", lambda e: e.memset(ones_f[:], 1.0), [], [ones_f])
    cs = pb.sb("cs", [128, 16])
    pb.dma("sync", cs[:], cTd[:], [cTd], cs)
    pb.act(None, cs[:], cs[:], AF.Silu, [cs], [cs])
    crep = pb.sb("crep", [128, 16, 128], BF16)
    for k in range(16):
        pb.act(None, crep[:, k, :], ones_f[:], AF.Copy, [ones_f, cs], [crep], scale=cs[:, k:k + 1])
    wv = wd_.t.rearrange("(k p) n -> p k n", p=128)
    wts = [pb.sb("mw%d" % i, [128, 16, 512], BF16) for i in range(2)]
    bss = [pb.sb("mbs%d" % i, [128, 512]) for i in range(2)]
    oss = [pb.sb("mos%d" % i, [128, 512]) for i in range(2)]
    for j in range(nchunk):
        wt, bs, os_, pst = wts[j % 2], bss[j % 2], oss[j % 2], banks[j % 2]
        pb.dma("gpsimd", wt[:], wv[:, :, j * 512:(j + 1) * 512], [wd_], wt)
        pb.dma("sync", bs[:], brepd[:, j * 512:(j + 1) * 512], [brepd], bs)
        for k in range(16):
            pb.mm(pst[:], crep[:, k, :], wt[:, k, :], k == 0, k == 15, [crep, wt], [pst])
        pb.op("vector", lambda e, os_=os_, pst=pst, bs=bs: e.tensor_tensor(out=os_[:], in0=pst[:], in1=bs[:], op=ALU.add),
              [pst, bs], [os_])
        pb.dma("sync", modrep[lbase + j // 4][:, (j % 4) * 512:(j % 4 + 1) * 512], os_[:], [os_], modrep)
        yield j


def emit_mod(pb, l, cTd, wd_, brepd, modrep, nchunk=24, lbase=None):
    if lbase is None:
        lbase = l * 6
    pb.begin({})
    for _ in mod_steps(pb, cTd, wd_, brepd, modrep, nchunk, lbase, pb.banks()[0:2]):
        pass
    pb.end()


def build_fused():
    pb = PB()
    V = lambda parent, ap: T(ap, parent.b)
    x = pb.din("x", [S, D])
    cT = pb.din("cT", [DEPTH * 0 + 128, 16])
    w_ada = pb.din("w_ada", [DEPTH, D, 6 * D])
    brep = pb.din("brep", [DEPTH, 128, 6 * D])
    w_in = pb.din("w_in", [DEPTH, D, INW])
    w_out = pb.din("w_out", [DEPTH, D, D])
    w_router = pb.din("w_router", [DEPTH, D, NE])
    w_gate = pb.din("w_gate", [DEPTH, NE, D, FF])
    w_up = pb.din("w_up", [DEPTH, NE, D, FF])
    w_down = pb.din("w_down", [DEPTH, NE, FF, D])
    cs5 = pb.din("cs5", [S, 640])
    sn5 = pb.din("sn5", [S, 640])
    gain = pb.din("gainr", [DEPTH, 128, 640])
    sinkr = pb.din("sinkrr", [DEPTH * 2, 128, 512])
    bg = pb.din("bgr", [DEPTH * 2, 128, 128])
    mgain = pb.din("mgainr", [DEPTH * 2, 128, 512])
    out = pb.dout("out", [S, D])
    modrep = pb.dscratch("modrep", [DEPTH * 6, 128, D])
    pre = pb.dscratch("pre", [S, INW])
    mixT = pb.dscratch("mixT", [16, 128, S], BF16)
    xa = pb.dscratch("xa", [S, D])
    xb = pb.dscratch("xb", [S, D])
    xc = pb.dscratch("xc", [S, D])
    h2 = pb.dscratch("h2", [S, D], BF16)
    aff = pb.dscratch("aff", [S, NE])
    affT = pb.dscratch("affT", [NE, S])
    parts = [pb.dscratch("part%d" % i, [S, D]) for i in range(2)]
    yscr = pb.dscratch("yscr", [16, 128, D], BF16)
    pb.banks()
    xin = x
    for l in range(DEPTH):
        emit_mod(pb, l, cT, V(w_ada, w_ada[l]), V(brep, brep[l]), modrep)
        M = lambda i: V(modrep, modrep[l * 6 + i])
        x3 = xa if l == 0 else xc
        for r in range(2):
            rs = slice(r * 1024, (r + 1) * 1024)
            b = {"x": V(xin, xin[rs, :]), "sc": M(1), "sh": M(0), "w": V(w_in, w_in[l]), "pre": V(pre, pre[rs, :])}
            if l > 0:
                b.update({"p0": V(parts[0], parts[0][rs, :]), "p1": V(parts[1], parts[1][rs, :]),
                          "g2": V(modrep, modrep[(l - 1) * 6 + 5]), "xo": V(xb, xb[rs, :])})
            pb.begin(b)
            build_L1(l > 0, pb)
            pb.end()
        if l > 0:
            xin = xb
        for r in range(2):
            b = {"aq": V(pre, pre[:, r * 512:(r + 1) * 512]), "ak": V(pre, pre[:, 1024 + r * 128:1024 + (r + 1) * 128]),
                 "cs": cs5, "sn": sn5, "gain": V(gain, gain[l]), "av": V(pre, pre[:, 1280 + r * 128:1280 + (r + 1) * 128]),
                 "sinkr": V(sinkr, sinkr[l * 2 + r]),
                 "mq": V(pre, pre[:, 1536 + r * 256:1536 + (r + 1) * 256]),
                 "mk": V(pre, pre[:, 2048 + r * 256:2048 + (r + 1) * 256]),
                 "mv": V(pre, pre[:, 2560 + r * 512:2560 + (r + 1) * 512]),
                 "mo": V(pre, pre[:, 3584 + r * 512:3584 + (r + 1) * 512]),
                 "gt16": V(pre, pre[:, 4608:4624]), "rsel": r,
                 "bg": V(bg, bg[l * 2 + r]), "mgain": V(mgain, mgain[l * 2 + r]),
                 "attT": V(mixT, mixT[4 * r:4 * r + 4]), "moT": V(mixT, mixT[8 + 4 * r:8 + 4 * r + 4])}
            pb.begin(b)
            build_L2(pb)
            pb.end()
        for r in range(2):
            rs = slice(r * 1024, (r + 1) * 1024)
            b = {"mixT": V(mixT, mixT[:, :, rs]), "x": V(xin, xin[rs, :]), "w": V(w_out, w_out[l]),
                 "g1": M(2), "sc": M(4), "sh": M(3), "wr": V(w_router, w_router[l]),
                 "x1": V(x3, x3[rs, :]), "h2": V(h2, h2[rs, :]), "aff": V(aff, aff[rs, :]), "affT": V(affT, affT[:, rs])}
            pb.begin(b)
            build_L3(pb)
            pb.end()
        for r in range(2):
            es = slice(8 * r, 8 * r + 8)
            b = {"h2": h2, "affT": V(affT, affT[es, :]), "afft": V(aff, aff[:, es]),
                 "wg": V(w_gate, w_gate[l, es]), "wu": V(w_up, w_up[l, es]), "wd": V(w_down, w_down[l, es]),
                 "part": parts[r], "yscr": yscr}
            pb.begin(b)
            build_L4(pb)
            pb.end()
        xin = x3
    for r in range(2):
        rs = slice(r * 1024, (r + 1) * 1024)
        b = {"x": V(xc, xc[rs, :]), "p0": V(parts[0], parts[0][rs, :]), "p1": V(parts[1], parts[1][rs, :]),
             "g2": V(modrep, modrep[(DEPTH - 1) * 6 + 5]), "xo": V(out, out[rs, :])}
        pb.begin(b)
        build_L5(pb)
        pb.end()
    pb.outs = [out]
    nc = pb.finish()
    return nc, pb.S.stats


GROUPS = [[0, 1], [2, 3], [4, 5], [6, 7]]


def coll_gather(pb, src, dst):
    return pb.S.op("gpsimd", lambda e: e.collective_compute("AllGather", ALU.bypass, replica_groups=GROUPS,
                                                            ins=[src.t.opt()], outs=[dst.t.opt()]),
                   reads=[src.b], writes=[dst.b], dma=True, dma_inc=1)


def emit_pack(pb, parto, selv, oselv, psend, pown):
    sel = pb.sb("sel", [128, 2])
    osel = pb.sb("osel", [128, 2])
    pb.dma("sync", sel[:], selv[:], [selv], sel)
    pb.dma("sync", osel[:], oselv[:], [oselv], osel)
    A = [pb.sb("pkA%d" % i, [128, D]) for i in range(2)]
    B = [pb.sb("pkB%d" % i, [128, D]) for i in range(2)]
    o1 = [pb.sb("pko%d" % i, [128, D]) for i in range(2)]
    o2 = [pb.sb("pkp%d" % i, [128, D]) for i in range(2)]
    for jj in range(8):
        a, b_, s1, s2 = A[jj % 2], B[jj % 2], o1[jj % 2], o2[jj % 2]
        rows = slice(jj * 128, (jj + 1) * 128)
        pb.dma("sync", a[:], parto[jj * 128:(jj + 1) * 128, :], [parto], a)
        pb.dma("sync", b_[:], parto[1024 + jj * 128:1024 + (jj + 1) * 128, :], [parto], b_)
        pb.op("vector", lambda e, a=a, s1=s1: e.tensor_scalar(out=s1[:], in0=a[:], scalar1=osel[:, 0:1], scalar2=None,
                                                              op0=ALU.mult), [a, osel], [s1])
        pb.op("vector", lambda e, b_=b_, s1=s1: e.scalar_tensor_tensor(out=s1[:], in0=b_[:], scalar=osel[:, 1:2], in1=s1[:],
                                                                       op0=ALU.mult, op1=ALU.add), [b_, osel, s1], [s1])
        pb.op("vector", lambda e, a=a, s2=s2: e.tensor_scalar(out=s2[:], in0=a[:], scalar1=sel[:, 0:1], scalar2=None,
                                                              op0=ALU.mult), [a, sel], [s2])
        pb.op("vector", lambda e, b_=b_, s2=s2: e.scalar_tensor_tensor(out=s2[:], in0=b_[:], scalar=sel[:, 1:2], in1=s2[:],
                                                                       op0=ALU.mult, op1=ALU.add), [b_, sel, s2], [s2])
        pb.dma("sync", psend[rows, :], s1[:], [s1], psend)
        pb.dma("sync", pown[rows, :], s2[:], [s2], pown)


def emit_recv_tiles(pb, osel, pown, precv, dst_tile, tmp_tiles, rows):
    pc, off = rows.start // 256, rows.start % 256
    pb.dma("sync", dst_tile[:], pown[rows, :], [pown], dst_tile)
    for q in range(2):
        tt = tmp_tiles[q]
        pb.dma("sync", tt[:], precv[pc].t[q, off:off + 128, :], [precv[pc]], tt)
        pb.op("vector", lambda e, tt=tt, q=q: e.scalar_tensor_tensor(
            out=dst_tile[:], in0=tt[:], scalar=osel[:, q:q + 1], in1=dst_tile[:], op0=ALU.mult, op1=ALU.add),
            [tt, osel, dst_tile], [dst_tile])


def emit_blend_tiles(pb, sel, partg_p, dst_tile, tmp_tiles, rows):
    first = True
    i = 0
    for q in range(2):
        for h in range(2):
            tt = tmp_tiles[i % len(tmp_tiles)]
            i += 1
            grow = h * 1024 + rows.start
            pc, off = grow // 256, grow % 256
            pb.dma("sync", tt[:], partg_p[pc].t[q, off:off + 128, :], [partg_p[pc]], tt)
            if first:
                pb.op("vector", lambda e, tt=tt, h=h: e.tensor_scalar(out=dst_tile[:], in0=tt[:], scalar1=sel[:, h:h + 1],
                                                                      scalar2=None, op0=ALU.mult), [tt, sel], [dst_tile])
                first = False
            else:
                pb.op("vector", lambda e, tt=tt, h=h: e.scalar_tensor_tensor(
                    out=dst_tile[:], in0=tt[:], scalar=sel[:, h:h + 1], in1=dst_tile[:], op0=ALU.mult, op1=ALU.add),
                    [tt, sel, dst_tile], [dst_tile])


def emit_h(pb, xsrc, scd, shd, hTs, prol=None):
    NT = 8
    PS = pb.banks()
    ident = make_ident(pb, BF16)
    onepsc = pb.sb("onepsc", [128, D])
    shs = pb.sb("shs", [128, D])
    pb.dma("sync", onepsc[:], scd[:], [scd], onepsc)
    pb.dma("sync", shs[:], shd[:], [shd], shs)
    pb.op("vector", lambda e: e.tensor_scalar(out=onepsc[:], in0=onepsc[:], scalar1=1.0, scalar2=None, op0=ALU.add),
          [onepsc], [onepsc])
    if prol is not None:
        partg, selv, g2d, xo = prol
        sel = pb.sb("sel", [128, 2])
        pb.dma("sync", sel[:], selv[:], [selv], sel)
        g2 = pb.sb("g2s", [128, D])
        pb.dma("sync", g2[:], g2d[:], [g2d], g2)
        tmps = [pb.sb("btmp%d" % i, [128, D]) for i in range(2)]
        pacc = pb.sb("pacc", [128, D])
    hTt = pb.sb("hTt", [128, 16, 1024], BF16)
    xts = [pb.sb("xt%d" % i, [128, D]) for i in range(2)]
    junk = pb.sb("junk", [128, D])
    hb = pb.sb("hb", [128, D], BF16)
    ssq = pb.sb("ssq", [128, 1])
    for t in range(NT):
        xt = xts[t % 2]
        rows = slice(t * 128, (t + 1) * 128)
        pb.dma("sync", xt[:], xsrc[rows, :], [xsrc], xt)
        if prol is not None:
            emit_recv_tiles(pb, sel, partg[0], partg[1], pacc, tmps, rows)
            pb.op("gpsimd", lambda e: e.tensor_tensor(out=pacc[:], in0=pacc[:], in1=g2[:], op=ALU.mult), [pacc, g2], [pacc])
            pb.op("vector", lambda e, xt=xt: e.tensor_tensor(out=xt[:], in0=xt[:], in1=pacc[:], op=ALU.add), [pacc, xt], [xt])
            pb.dma("sync", xo[rows, :], xt[:], [xt], xo)
        if hTs is None:
            continue
        emit_norm_mod(pb, xt, onepsc, shs, junk, ssq, hb)
        for g in range(4):
            pt_ = PS[4 + g % 2]
            for q in range(4):
                k = g * 4 + q
                pb.mm(pt_[:, q * 128:(q + 1) * 128], hb[:, k * 128:(k + 1) * 128], ident[:], True, True, [hb, ident], [pt_])
            pb.act(None, hTt[:, g * 4:(g + 1) * 4, t * 128:(t + 1) * 128],
                   pt_[:].rearrange("p (q n) -> p q n", q=4), AF.Copy, [pt_], [hTt])
    if hTs is not None:
        pb.dma("sync", hTs.t.rearrange("k p t -> p k t"), hTt[:], [hTt], hTs)


PREW = 2312


def emit_inproj(pb, hTg, w, pre):
    for _ in inproj_steps(pb, hTg, w, pre):
        pass


def inproj_steps(pb, hTg, w, pre):
    PS = pb.banks()
    hT = pb.sb("hT", [128, 16, S], BF16)
    for q in range(2):
        for pc in range(2):
            pb.dma("sync", hT[:, pc * 8:(pc + 1) * 8, q * 1024:(q + 1) * 1024],
                   hTg[pc].t[q].rearrange("k p t -> p k t"), [hTg[pc]], hT)
    wv = w.t.rearrange("(k p) n -> p k n", p=128)
    wts = [pb.sb("w%d" % i, [128, 16, 512], BF16) for i in range(2)]
    stg = [pb.sb("stg%d" % i, [128, 512]) for i in range(4)]
    cnt = 0
    for j in range(5):
        c0 = j * 512
        cw = min(512, PREW - c0)
        wt = wts[j % 2]
        pb.dma("gpsimd", wt[:, :, :cw], wv[:, :, c0:c0 + cw], [w], wt)
        for t in range(16):
            pst = PS[cnt % 4]
            st = stg[cnt % 4]
            for k in range(16):
                pb.mm(pst[:, :cw], hT[:, k, t * 128:(t + 1) * 128], wt[:, k, :cw], k == 0, k == 15, [hT, wt], [pst])
            if cnt % 2 == 0:
                pb.op("vector", lambda e, st=st, pst=pst, cw=cw: e.tensor_copy(out=st[:, :cw], in_=pst[:, :cw]), [pst], [st])
            else:
                pb.act(None, st[:, :cw], pst[:, :cw], AF.Copy, [pst], [st])
            pb.dma("sync", pre[t * 128:(t + 1) * 128, c0:c0 + cw], st[:, :cw], [st], pre)
            cnt += 1
        yield j


def emit_affblend(pb, affg, affTg, selv, afft_m, affT_m):
    sel = pb.sb("sel", [128, 2])
    pb.dma("sync", sel[:], selv[:], [selv], sel)
    a = pb.sb("affa", [128, 16, 16])
    pb.dma("sync", a[:], affg.t.rearrange("q (j p) e -> p (q j) e", p=128), [affg], a)
    am = pb.sb("affm", [128, 16, 8])
    pb.op("vector", lambda e: e.tensor_scalar(out=am[:], in0=a[:, :, 0:8], scalar1=sel[:, 0:1], scalar2=None, op0=ALU.mult),
          [a, sel], [am])
    pb.op("vector", lambda e: e.scalar_tensor_tensor(out=am[:], in0=a[:, :, 8:16], scalar=sel[:, 1:2], in1=am[:],
                                                      op0=ALU.mult, op1=ALU.add), [a, sel, am], [am])
    pb.dma("sync", afft_m.t.rearrange("(j p) e -> p j e", p=128), am[:], [am], afft_m)
    tl = pb.sb("affTl", [8, 2, 1024])
    th = pb.sb("affTh", [8, 2, 1024])
    pb.dma("sync", tl[:], affTg.t[:, 0:8, :].rearrange("q e t -> e q t"), [affTg], tl)
    pb.dma("sync", th[:], affTg.t[:, 8:16, :].rearrange("q e t -> e q t"), [affTg], th)
    tm = pb.sb("affTm", [8, 2, 1024])
    pb.op("vector", lambda e: e.tensor_scalar(out=tm[:], in0=tl[:], scalar1=sel[0:8, 0:1], scalar2=None, op0=ALU.mult),
          [tl, sel], [tm])
    pb.op("vector", lambda e: e.scalar_tensor_tensor(out=tm[:], in0=th[:], scalar=sel[0:8, 1:2], in1=tm[:],
                                                      op0=ALU.mult, op1=ALU.add), [th, sel, tm], [tm])
    pb.dma("sync", affT_m.t.rearrange("e (q t) -> e q t", q=2), tm[:], [tm], affT_m)


def build_fused8():
    pb = PB()
    V = lambda parent, ap: T(ap, parent.b)
    x = pb.din("x", [1024, D])
    cT = pb.din("cT", [128, 16])
    selv = pb.din("selv", [128, 2])
    w_ada = pb.din("w_ada", [DEPTH, D, 3 * D])
    brep = pb.din("brep", [DEPTH, 128, 3 * D])
    oselv = pb.din("oselv", [128, 2])
    w_in = pb.din("w_in", [DEPTH, D, PREW])
    w_out = pb.din("w_out", [DEPTH, D, D])
    w_router = pb.din("w_router", [DEPTH, D, NE])
    w_gate = pb.din("w_gate", [DEPTH, 8, D, FF])
    w_up = pb.din("w_up", [DEPTH, 8, D, FF])
    w_down = pb.din("w_down", [DEPTH, 8, FF, D])
    cs5 = pb.din("cs5", [S, 640])
    sn5 = pb.din("sn5", [S, 640])
    gain = pb.din("gainr", [DEPTH, 128, 640])
    sinkr = pb.din("sinkrr", [DEPTH, 128, 512])
    bg = pb.din("bgr", [DEPTH, 128, 128])
    mgain = pb.din("mgainr", [DEPTH, 128, 512])
    out = pb.dout("out", [1024, D])
    modh = pb.dscratch("modh", [3, 128, D])
    modg = [[pb.dscratch("modg%d_%d" % (l, i), [2, 128, D]) for i in range(3)] for l in range(DEPTH)]
    psend = pb.dscratch("psend", [1024, D])
    pown = pb.dscratch("pown", [1024, D])
    precv = [pb.dscratch("precv%d" % i, [2, 256, D]) for i in range(4)]
    hTs = pb.dscratch("hTs", [16, 128, 1024], BF16)
    hTg = [pb.dscratch("hTg%d" % i, [2, 8, 128, 1024], BF16) for i in range(2)]
    pre = pb.dscratch("pre", [S, PREW])
    mixo = pb.dscratch("mixo", [8, 128, S], BF16)
    mixg = [pb.dscratch("mixg%d" % i, [2, 4, 128, S], BF16) for i in range(2)]
    xa = pb.dscratch("xa", [1024, D])
    xb = pb.dscratch("xb", [1024, D])
    xc = pb.dscratch("xc", [1024, D])
    h2o = pb.dscratch("h2o", [1024, D], BF16)
    h2g = [pb.dscratch("h2g%d" % i, [2, 512, D], BF16) for i in range(2)]
    affo = pb.dscratch("affo", [1024, NE])
    affg = pb.dscratch("affg", [2, 1024, NE])
    affTo = pb.dscratch("affTo", [NE, 1024])
    affTg = pb.dscratch("affTg", [2, NE, 1024])
    afft_m = pb.dscratch("afftm", [S, 8])
    affT_m = pb.dscratch("affTm", [8, S])
    parto = pb.dscratch("parto", [S, D])
    partg = [pb.dscratch("partg%d" % i, [2, 256, D]) for i in range(8)]
    yscr = pb.dscratch("yscr", [16, 128, D], BF16)
    pb.banks()
    rstate = {"iota": pb.sb("r_iota", [128, 256]), "identb": pb.sb("r_identb", [128, 128], BF16),
              "gm": pb.sb("r_gm", [128, 16, 8]), "pos": pb.sb("r_pos", [128, 16, 8])}
    xin = x
    for l in range(DEPTH):
        if l == 0:
            emit_mod(pb, l, cT, V(w_ada, w_ada[l]), V(brep, brep[l]), modh, nchunk=12, lbase=0)
            pb.begin({})
            for i in range(3):
                coll_gather(pb, V(modh, modh[i]), V(modg[l][i], modg[l][i].t.rearrange("q p d -> (q p) d")))
            pb.end()
        Ml = lambda ll, i: V(modg[ll][i % 3], modg[ll][i % 3].t[i // 3])
        M = lambda i: Ml(l, i)
        x3 = xa if l == 0 else xc
        pb.begin({})
        prol = None if l == 0 else ((pown, precv), oselv, Ml(l - 1, 5), xb)
        emit_h(pb, xin, M(1), M(0), hTs, prol)
        pb.end()
        if l > 0:
            xin = xb
        pb.begin({})
        for pc in range(2):
            coll_gather(pb, V(hTs, hTs.t.rearrange("k p t -> (k p) t")[pc * 1024:(pc + 1) * 1024, :]),
                        V(hTg[pc], hTg[pc].t.rearrange("q k p t -> (q k p) t")))
        if l + 1 < DEPTH:
            g1 = inproj_steps(pb, hTg, V(w_in, w_in[l]), pre)
            g2 = mod_steps(pb, cT, V(w_ada, w_ada[l + 1]), V(brep, brep[l + 1]), modh, 12, 0, pb.banks()[6:8])
            for n2 in (3, 3, 2, 2, 2):
                next(g1)
                for _ in range(n2):
                    next(g2)
            for _ in g1:
                pass
            for _ in g2:
                pass
        else:
            emit_inproj(pb, hTg, V(w_in, w_in[l]), pre)
        pb.end()
        if l + 1 < DEPTH:
            pb.begin({})
            for i in range(3):
                coll_gather(pb, V(modh, modh[i]), V(modg[l + 1][i], modg[l + 1][i].t.rearrange("q p d -> (q p) d")))
            pb.end()
        b = {"aq": V(pre, pre[:, 0:512]), "ak": V(pre, pre[:, 512:640]), "cs": cs5, "sn": sn5, "gain": V(gain, gain[l]),
             "av": V(pre, pre[:, 640:768]), "sinkr": V(sinkr, sinkr[l]),
             "mq": V(pre, pre[:, 768:1024]), "mk": V(pre, pre[:, 1024:1280]), "mv": V(pre, pre[:, 1280:1792]),
             "mo": V(pre, pre[:, 1792:2304]), "gt": V(pre, pre[:, 2304:2312]),
             "bg": V(bg, bg[l]), "mgain": V(mgain, mgain[l]),
             "attT": V(mixo, mixo[0:4]), "moT": V(mixo, mixo[4:8]), "mixo": mixo, "mixg": mixg}
        pb.begin(b)
        build_L2(pb)
        pb.end()
        b = {"mixg": mixg, "selv": selv, "x": xin, "w": V(w_out, w_out[l]),
             "g1": M(2), "sc": M(4), "sh": M(3), "wr": V(w_router, w_router[l]),
             "x1": x3, "h2": h2o, "aff": affo, "affT": affTo}
        pb.begin(b)
        build_L3(pb)
        pb.end()
        pb.begin({})
        coll_gather(pb, affo, V(affg, affg.t.rearrange("q t e -> (q t) e")))
        coll_gather(pb, affTo, V(affTg, affTg.t.rearrange("q e t -> (q e) t")))
        pb.end()
        pb.begin({})
        emit_affblend(pb, affg, affTg, selv, afft_m, affT_m)
        pb.end()
        b = {"h2p": h2g, "affT": affT_m, "afft": afft_m,
             "wg": V(w_gate, w_gate[l]), "wu": V(w_up, w_up[l]), "wd": V(w_down, w_down[l]),
             "part": parto, "yscr": yscr, "rstate": rstate, "psend": psend, "pown": pown, "precv": precv,
             "selv": selv, "oselv": oselv}
        pb.begin(b)
        for pc in range(2):
            coll_gather(pb, V(h2o, h2o[pc * 512:(pc + 1) * 512, :]), V(h2g[pc], h2g[pc].t.rearrange("q t d -> (q t) d")))
        build_L4(pb)
        pb.end()
        xin = x3
    pb.begin({})
    emit_h(pb, xc, Ml(0, 0), Ml(0, 0), None, ((pown, precv), oselv, Ml(DEPTH - 1, 5), out))
    pb.end()
    pb.outs = [out]
    nc = pb.finish()
    return nc, pb.S.stats


def in_cols(r):
    ar = np.arange
    gates = (4608 + (ar(4)[:, None] * 4 + (2 * r + ar(2))[None, :])).reshape(-1)
    return np.concatenate([ar(r * 512, (r + 1) * 512), 1024 + ar(r * 128, (r + 1) * 128), 1280 + ar(r * 128, (r + 1) * 128),
                           1536 + ar(r * 256, (r + 1) * 256), 2048 + ar(r * 256, (r + 1) * 256),
                           2560 + ar(r * 512, (r + 1) * 512), 3584 + ar(r * 512, (r + 1) * 512), gates])


_NC = {}


def kernel(x, c, w_ada, b_ada, w_in, b_gates, q_gain, k_gain, sink, m_gain, w_out, w_router, w_gate, w_up, w_down):
    f = lambda a: np.ascontiguousarray(np.asarray(a, dtype=np.float32))
    x, c, w_ada, b_ada, w_in, b_gates = f(x), f(c), f(w_ada), f(b_ada), f(w_in), f(b_gates)
    q_gain, k_gain, sink, m_gain, w_out, w_router = f(q_gain), f(k_gain), f(sink), f(m_gain), f(w_out), f(w_router)
    w_gate, w_up, w_down = f(w_gate), f(w_up), f(w_down)
    if "nc" not in _NC:
        _NC["nc"] = build_fused8()[0]
    nc = _NC["nc"]
    cs5, sn5 = rope_tables_np()
    brep = np.stack([rep128(b_ada[l]) for l in range(DEPTH)])
    gainr = np.stack([rep128(np.concatenate([np.tile(q_gain[l], 4), k_gain[l]])) for l in range(DEPTH)])
    per_r = []
    for r in range(2):
        es = slice(8 * r, 8 * r + 8)
        selv = np.zeros((128, 2), np.float32)
        selv[:, r] = 1.0
        per_r.append({
            "selv": selv, "oselv": np.ascontiguousarray(1.0 - selv),
            "w_ada": np.ascontiguousarray(w_ada[:, :, r * 3 * D:(r + 1) * 3 * D]),
            "brep": np.stack([rep128(b_ada[l][r * 3 * D:(r + 1) * 3 * D]) for l in range(DEPTH)]),
            "w_in": np.ascontiguousarray(w_in[:, :, in_cols(r)]),
            "w_gate": np.ascontiguousarray(w_gate[:, es]), "w_up": np.ascontiguousarray(w_up[:, es]),
            "w_down": np.ascontiguousarray(w_down[:, es]),
            "sinkrr": np.stack([rep128(np.repeat(sink[l][4 * r:4 * r + 4], 128)) for l in range(DEPTH)]),
            "bgr": np.stack([rep128(np.tile(b_gates[l][:, 2 * r:2 * r + 2].reshape(8), 16)) for l in range(DEPTH)]),
            "mgainr": np.stack([rep128(m_gain[l][r * 512:(r + 1) * 512]) for l in range(DEPTH)]),
        })
    maps = []
    for i in range(8):
        b, r = i // 2, i % 2
        m = {"x": np.ascontiguousarray(x[b, r * 1024:(r + 1) * 1024]), "cT": np.ascontiguousarray(c[b].reshape(16, 128).T),
             "w_out": w_out, "w_router": w_router, "cs5": cs5, "sn5": sn5, "gainr": gainr}
        m.update(per_r[r])
        maps.append(m)
    res = run(nc, maps)
    out = np.zeros((NB, S, D), np.float32)
    for i in range(8):
        b, r = i // 2, i % 2
        out[b, r * 1024:(r + 1) * 1024] = res[i]["out"]
    return out
```
